# Optimizing a Trainium2 kernel written in Bass

```python
import math
import jax, jax.numpy as jnp
from jax import lax
import numpy as np

D_MODEL = 1024
BATCH = 4
SEQ = 8192
DEPTH = 4

N_MIXERS = 2
N_HEADS = 4
QK_DIM = D_MODEL // (2 * N_HEADS)
V_DIM = D_MODEL // N_HEADS
CHUNK = 128
GATE_CAP = 15.0
CONV_WIDTH = 3
D_FF = 4 * D_MODEL
EPS = 1e-6
N_MLSTM_LAYERS = (DEPTH + 1) // 2
N_CONV_LAYERS = DEPTH // 2
QK_COLS = N_HEADS * QK_DIM
MLSTM_IN = 2 * QK_COLS + 2 * D_MODEL + 2 * N_HEADS
CONV_IN = 3 * D_MODEL

kernel_name = "hybrid_mlstm_shortconv_sqrelu"


def rms_norm(x, g):
    xf = x.astype(jnp.float32)
    y = xf * lax.rsqrt(jnp.mean(xf * xf, axis=-1, keepdims=True) + EPS)
    return (y * g.astype(jnp.float32)).astype(x.dtype)


def mlstm_mixer(x, w_in, b_gates, g_hnorm, w_out):
    B_, S, _ = x.shape
    nc = S // CHUNK
    proj = x @ w_in
    q, k, v, o, gates = jnp.split(
        proj, [QK_COLS, 2 * QK_COLS, 2 * QK_COLS + D_MODEL, 2 * QK_COLS + 2 * D_MODEL], axis=-1)
    f32 = jnp.float32
    q = q.astype(f32).reshape(B_, S, N_HEADS, QK_DIM).transpose(0, 2, 1, 3)
    k = (k.astype(f32) * (QK_DIM ** -0.5)).reshape(B_, S, N_HEADS, QK_DIM).transpose(0, 2, 1, 3)
    v = v.astype(f32).reshape(B_, S, N_HEADS, V_DIM).transpose(0, 2, 1, 3)
    gates = gates.astype(f32) + b_gates.astype(f32)
    gates = GATE_CAP * jnp.tanh(gates / GATE_CAP)
    ig = gates[..., :N_HEADS].transpose(0, 2, 1)
    logf = jax.nn.log_sigmoid(gates[..., N_HEADS:]).transpose(0, 2, 1)

    qc = q.reshape(B_, N_HEADS, nc, CHUNK, QK_DIM)
    kc = k.reshape(B_, N_HEADS, nc, CHUNK, QK_DIM)
    vc = v.reshape(B_, N_HEADS, nc, CHUNK, V_DIM)
    igc = ig.reshape(B_, N_HEADS, nc, CHUNK)
    bcum = jnp.cumsum(logf.reshape(B_, N_HEADS, nc, CHUNK), axis=-1)
    b_last = bcum[..., -1]

    log_w = b_last[..., None] - bcum + igc
    m_loc = jnp.max(log_w, axis=-1)
    w = jnp.exp(log_w - m_loc[..., None])
    kv_loc = jnp.einsum('bhcl,bhcld,bhcle->bhcde', w, kc, vc)
    n_loc = jnp.einsum('bhcl,bhcld->bhcd', w, kc)

    def step(carry, inp):
        C, n, m = carry
        bl, ml, kvl, nl = inp
        m_new = jnp.maximum(bl + m, ml)
        a = jnp.exp(bl + m - m_new)
        s = jnp.exp(ml - m_new)
        C_new = a[..., None, None] * C + s[..., None, None] * kvl
        n_new = a[..., None] * n + s[..., None] * nl
        return (C_new, n_new, m_new), (C, n, m)

    init = (jnp.zeros((B_, N_HEADS, QK_DIM, V_DIM), f32),
            jnp.zeros((B_, N_HEADS, QK_DIM), f32),
            jnp.zeros((B_, N_HEADS), f32))
    xs = (jnp.moveaxis(b_last, 2, 0), jnp.moveaxis(m_loc, 2, 0),
          jnp.moveaxis(kv_loc, 2, 0), jnp.moveaxis(n_loc, 2, 0))
    _, (C_prev, n_prev, m_prev) = lax.scan(step, init, xs)
    C_prev = jnp.moveaxis(C_prev, 0, 2)
    n_prev = jnp.moveaxis(n_prev, 0, 2)
    m_prev = jnp.moveaxis(m_prev, 0, 2)

    causal = jnp.tril(jnp.ones((CHUNK, CHUNK), dtype=bool))
    logD = bcum[..., :, None] - bcum[..., None, :] + igc[..., None, :]
    logD = jnp.where(causal, logD, -jnp.inf)
    log_inter = bcum + m_prev[..., None]
    m_t = jnp.maximum(jnp.max(logD, axis=-1), log_inter)
    Dm = jnp.exp(logD - m_t[..., None])
    scores = jnp.einsum('bhcld,bhcsd->bhcls', qc, kc) * Dm
    inter = jnp.exp(log_inter - m_t)
    num = (jnp.einsum('bhcls,bhcse->bhcle', scores, vc)
           + inter[..., None] * jnp.einsum('bhcld,bhcde->bhcle', qc, C_prev))
    den = jnp.sum(scores, axis=-1) + inter * jnp.einsum('bhcld,bhcd->bhcl', qc, n_prev)
    h = num / jnp.maximum(jnp.abs(den), jnp.exp(-m_t))[..., None]

    h = h.reshape(B_, N_HEADS, S, V_DIM).transpose(0, 2, 1, 3)
    h = h * lax.rsqrt(jnp.mean(h * h, axis=-1, keepdims=True) + EPS)
    h = h * g_hnorm.astype(f32).reshape(N_HEADS, V_DIM)
    h = jax.nn.sigmoid(o.astype(f32)) * h.reshape(B_, S, D_MODEL)
    return h.astype(x.dtype) @ w_out


def short_conv_mixer(x, w_in, w_conv, w_out):
    proj = x @ w_in
    bgate, cgate, xh = jnp.split(proj, 3, axis=-1)
    u = cgate * xh
    conv = lax.conv_general_dilated(
        u, w_conv[:, None, :].astype(u.dtype), window_strides=(1,),
        padding=[(CONV_WIDTH - 1, 0)],
        dimension_numbers=('NWC', 'WIO', 'NWC'),
        feature_group_count=D_MODEL)
    return (bgate * conv) @ w_out


def sqrelu_mlp(x, w_up, w_down):
    h = jax.nn.relu(x @ w_up)
    return (h * h) @ w_down


def setup_inputs(seed: int = 0) -> dict:
    key = jax.random.key(seed)
    ks = jax.random.split(key, 12)
    f32 = jnp.float32
    x = jax.random.normal(ks[0], (BATCH, SEQ, D_MODEL), f32)
    norm_g = 1.0 + 0.05 * jax.random.normal(ks[1], (DEPTH, 4, D_MODEL), f32)
    w_in_mlstm = jax.random.normal(ks[2], (N_MLSTM_LAYERS, D_MODEL, MLSTM_IN), f32) * D_MODEL ** -0.5
    i_bias = -3.0 + 0.1 * jax.random.normal(ks[3], (N_MLSTM_LAYERS, N_HEADS), f32)
    f_bias = (jnp.linspace(3.0, 6.0, N_HEADS, dtype=f32)[None, :]
              + 0.1 * jax.random.normal(ks[4], (N_MLSTM_LAYERS, N_HEADS), f32))
    b_gates_mlstm = jnp.concatenate([i_bias, f_bias], axis=-1)
    g_hnorm = 1.0 + 0.05 * jax.random.normal(ks[5], (N_MLSTM_LAYERS, D_MODEL), f32)
    w_out_mlstm = jax.random.normal(ks[6], (N_MLSTM_LAYERS, D_MODEL, D_MODEL), f32) * D_MODEL ** -0.5
    w_in_conv = jax.random.normal(ks[7], (N_CONV_LAYERS, D_MODEL, CONV_IN), f32) * D_MODEL ** -0.5
    w_conv = jax.random.normal(ks[8], (N_CONV_LAYERS, CONV_WIDTH, D_MODEL), f32) * CONV_WIDTH ** -0.5
    w_out_conv = jax.random.normal(ks[9], (N_CONV_LAYERS, D_MODEL, D_MODEL), f32) * D_MODEL ** -0.5
    w_mlp_up = jax.random.normal(ks[10], (DEPTH, D_MODEL, D_FF), f32) * D_MODEL ** -0.5
    w_mlp_down = jax.random.normal(ks[11], (DEPTH, D_FF, D_MODEL), f32) * D_FF ** -0.5
    return {"x": x, "norm_g": norm_g, "w_in_mlstm": w_in_mlstm, "b_gates_mlstm": b_gates_mlstm,
            "g_hnorm": g_hnorm, "w_out_mlstm": w_out_mlstm, "w_in_conv": w_in_conv,
            "w_conv": w_conv, "w_out_conv": w_out_conv, "w_mlp_up": w_mlp_up,
            "w_mlp_down": w_mlp_down}


def reference(x, norm_g, w_in_mlstm, b_gates_mlstm, g_hnorm, w_out_mlstm, w_in_conv,
              w_conv, w_out_conv, w_mlp_up, w_mlp_down):
    for i in range(DEPTH):
        g = norm_g[i]
        j = i // N_MIXERS
        h = rms_norm(x, g[0])
        if i % N_MIXERS == 0:
            h = mlstm_mixer(h, w_in_mlstm[j], b_gates_mlstm[j], g_hnorm[j], w_out_mlstm[j])
        else:
            h = short_conv_mixer(h, w_in_conv[j], w_conv[j], w_out_conv[j])
        x = x + rms_norm(h, g[1])
        h = sqrelu_mlp(rms_norm(x, g[2]), w_mlp_up[i], w_mlp_down[i])
        x = x + rms_norm(h, g[3])
    return x
```

```python
import contextlib
import numpy as np
import concourse.bass as bass
import concourse.mybir as mybir
from concourse.bass_utils import run_bass_kernel_spmd

F32 = mybir.dt.float32
F32R = mybir.dt.float32r
BF16 = mybir.dt.bfloat16
AF = mybir.ActivationFunctionType
ALU = mybir.AluOpType

D = 1024
KC = 8
NH = 4
DFF = 4096
EPS = 1e-6
CAP = 15.0
NCORES = 8
BLK = 512
CH = 128
TILE_EL = 2048
NB = 4
PREFETCH = False
TILES_PER_LAYER = 48
VA = 257
CELL = 256


CP_IDENT = 0
CP_MASK = 128
CP_ONES = 256
CP_G = 384
CP_WC = 512
CP_NEGHALF = 560
CP_DIAG4 = 562
CP_NEGDIAG4 = 566
CP_BG = 570
CP_FLAG = 574
CP_COLS = 576


class View:
    __slots__ = ("ap", "space", "base", "fshape", "fstr", "esz")

    def __init__(self, ap, space, base, fshape, fstr, esz):
        self.ap = ap
        self.space = space
        self.base = base
        self.fshape = list(fshape)
        self.fstr = list(fstr)
        self.esz = esz

    @property
    def lo(self):
        return self.base

    @property
    def hi(self):
        return self.base + (sum((n - 1) * s for n, s in zip(self.fshape, self.fstr)) + 1) * self.esz

    def __getitem__(self, idx):
        if not isinstance(idx, tuple):
            idx = (idx,)
        idx = idx + (slice(None),) * (len(self.fshape) - len(idx))
        base = self.base
        fshape, fstr = [], []
        for i, n, s in zip(idx, self.fshape, self.fstr):
            if isinstance(i, int):
                base += i * s * self.esz
            else:
                st, sp, step = i.indices(n)
                assert step == 1
                base += st * s * self.esz
                fshape.append(sp - st)
                fstr.append(s)
        return View(self.ap[(slice(None),) + idx], self.space, base, fshape, fstr, self.esz)

    def p(self, p0, p1):
        return View(self.ap[p0:p1], self.space, self.base, self.fshape, self.fstr, self.esz)

    def w(self, ap):
        return View(ap, self.space, self.base, self.fshape, self.fstr, self.esz)


class Ins:
    __slots__ = ("q", "fn", "raw", "oth", "sig", "epos", "signaled", "ev", "waits", "gidx", "tag")

    def __init__(self, q, fn, sig):
        self.q = q
        self.fn = fn
        self.raw = set()
        self.oth = set()
        self.sig = sig
        self.signaled = False
        self.ev = None
        self.waits = []


class Prog:
    CELLS = {"sb": CELL, "ps": 2048}

    def __init__(self):
        self.all = []
        self.byq = {q: [] for q in ("pe", "act", "dve", "pool", "sp")}
        self.lastw = {"sb": {}, "ps": {}}
        self.readers = {"sb": {}, "ps": {}}
        self.last_sig_pos = {q: -1 for q in self.byq}
        self.cur_tag = "init"

    def _stream(self, ins):
        return ins.q if ins.sig == "eng" or ins.sig is None else ins.sig[1]

    def _cells(self, v):
        cs = self.CELLS[v.space]
        return range(v.lo // cs, (v.hi - 1) // cs + 1)

    def add(self, q, fn, reads=(), writes=(), sig="eng", deps=(), force_signal=None):
        ins = Ins(q, fn, sig)
        ins.epos = len(self.byq[q])
        ins.gidx = len(self.all)
        ins.tag = self.cur_tag
        for v in reads:
            lw, rd = self.lastw[v.space], self.readers[v.space]
            ps_as_write = v.space == "ps"
            for c in self._cells(v):
                w = lw.get(c)
                if w is not None:
                    ins.raw.add(w)
                if ps_as_write:
                    for r in rd.get(c, {}).values():
                        ins.oth.add(r)
                    lw[c] = ins
                    rd[c] = {}
                else:
                    rd.setdefault(c, {})[self._stream(ins)] = ins
        for v in writes:
            lw, rd = self.lastw[v.space], self.readers[v.space]
            for c in self._cells(v):
                w = lw.get(c)
                if w is not None:
                    ins.oth.add(w)
                for r in rd.get(c, {}).values():
                    ins.oth.add(r)
                lw[c] = ins
                rd[c] = {}
        for d in deps:
            ins.raw.add(d)
        ins.raw.discard(ins)
        ins.oth.discard(ins)
        keep = set()
        for d in ins.raw:
            if d.q == ins.q and d.sig == "eng" and ins.sig in ("eng", None) and ins.q == "pe":
                continue
            keep.add(d)
        for d in ins.oth:
            if d.q == ins.q and d.sig == "eng" and ins.sig in ("eng", None):
                continue
            keep.add(d)
        ins.raw = keep
        ins.oth = set()
        if sig == "eng":
            if q != "pe" or force_signal:
                ins.signaled = True
        elif sig is not None:
            ins.signaled = True
        self.all.append(ins)
        self.byq[q].append(ins)
        if ins.signaled and sig == "eng":
            self.last_sig_pos[q] = ins.epos
        for d in ins.raw:
            if d.sig == "eng" and not d.signaled and self.last_sig_pos[d.q] < d.epos:
                last = self.byq[d.q][-1]
                if last.sig != "eng":
                    raise RuntimeError("cannot signal")
                last.signaled = True
                self.last_sig_pos[d.q] = last.epos
        return ins

    def finalize(self):
        self.nextsig = {}
        for q, lst in self.byq.items():
            cnt = 0
            for i in lst:
                if i.sig == "eng" and i.signaled:
                    cnt += 1
                    i.ev = ("E_" + q, cnt)
            nxt = None
            arr = [None] * len(lst)
            for pos in range(len(lst) - 1, -1, -1):
                if lst[pos].sig == "eng" and lst[pos].signaled:
                    nxt = lst[pos]
                arr[pos] = nxt
            self.nextsig[q] = arr
        dmacnt = {}
        for i in self.all:
            if i.sig not in ("eng", None):
                _, sem, amt = i.sig
                dmacnt[sem] = dmacnt.get(sem, 0) + amt
                i.ev = (sem, dmacnt[sem])
        clock = {q: {} for q in self.byq}
        snap = {}
        nwaits = 0
        for i in self.all:
            ck = clock[i.q]
            need = {}
            for d in i.raw:
                if d.sig == "eng":
                    r = self.nextsig[d.q][d.epos]
                    assert r is not None, "unsignaled dependency"
                else:
                    r = d
                s, v = r.ev
                if ck.get(s, 0) >= v:
                    continue
                if need.get(s, (0, None))[0] < v:
                    need[s] = (v, r)
            for s, (v, r) in need.items():
                if ck.get(s, 0) >= v:
                    continue
                i.waits.append((s, v))
                nwaits += 1
                for s2, v2 in snap[r.ev].items():
                    if ck.get(s2, 0) < v2:
                        ck[s2] = v2
            if i.ev is not None:
                sn = dict(ck)
                sn[i.ev[0]] = i.ev[1]
                snap[i.ev] = sn
        self.nwaits = nwaits

    def emit(self, q, e, sems):
        for i in self.byq[q]:
            for s, v in i.waits:
                e.wait_ge(sems[s], v)
            if i.fn is None:
                continue
            bi = i.fn(e)
            if i.ev is not None:
                amt = 1 if i.sig == "eng" else i.sig[2]
                bi.then_inc(sems[i.ev[0]], amt)

    def sem_names(self):
        names = set()
        for i in self.all:
            if i.ev is not None:
                names.add(i.ev[0])
        return sorted(names)


class K:
    def __init__(self, nc, TOK, layers):
        self.nc = nc
        self.TOK = TOK
        self.layers = layers
        self.NBLK = TOK // BLK
        self.P = Prog()
        self.steps = []
        self.bank_rr = 0

    def setup_memory(self, S, PS):
        self.S = S
        self.PS = PS
        off = 0

        def take(nbytes):
            nonlocal off
            o = off
            off += (nbytes + CELL - 1) // CELL * CELL
            return o

        self.o_xt = take(KC * self.TOK * 4)
        self.o_cp = take(CP_COLS * 4)
        self.o_idb = take(512)
        self.o_ghn = take(D * 4)
        self.o_wg = take(512)
        self.o_wb = [take(TILE_EL * 2) for _ in range(NB)]
        self.o_persist = take(1024)
        self.o_arena = off
        self.total = off + self.ARENA
        TOK = self.TOK
        self.xT = self.mk(self.o_xt, F32, [KC, TOK])
        self.cp = self.mk(self.o_cp, F32, [CP_COLS])
        self.identb = self.mk(self.o_idb, BF16, [128])
        self.onesb = self.mk(self.o_idb + 256, BF16, [128])
        self.ghn = self.mk(self.o_ghn, F32, [D])
        self.wgs = [self.mk(self.o_wg, BF16, [KC, 8]), self.mk(self.o_wg + 256, BF16, [KC, 8])]
        self.wb_proj = [self.mk(o, BF16, [KC, 256]) for o in self.o_wb]
        self.wb_down = [self.mk(o, BF16, [16, 128]) for o in self.o_wb]
        pp = self.o_persist
        self.mpv = self.mk(pp, F32, [40])
        self.uprev = self.mk(pp + 256, F32, [KC, 2])
        self.min_ = self.mk(pp + 512, F32, [4])
        self.ident = self.cp[CP_IDENT:CP_IDENT + 128]
        self.maskneg = self.cp[CP_MASK:CP_MASK + 128]
        self.ones = self.cp[CP_ONES:CP_ONES + 128]
        self.neghalf = self.cp[CP_NEGHALF:CP_NEGHALF + 1]
        self.diag4 = self.cp[CP_DIAG4:CP_DIAG4 + 4].p(0, 4)
        self.negdiag4 = self.cp[CP_NEGDIAG4:CP_NEGDIAG4 + 4].p(0, 4)
        self.flag = self.cp[CP_FLAG:CP_FLAG + 1]
        a = self.o_arena
        self.RSTD = self.mk(a, F32, [BLK])
        self.TB = self.mk(a + 2048, F32, [BLK])
        self.SQ = [self.mk(a + 4096, F32, [BLK]), self.mk(a + 6144, F32, [BLK])]
        self.SQB = [self.mk(a + 4096, BF16, [BLK]), self.mk(a + 6144, BF16, [BLK])]
        self.HT = self.mk(a + 8192, BF16, [KC, BLK])
        b = a + 16384
        self.ACTT = self.mk(b, BF16, [16, BLK])
        self.YT_mlp = self.mk(b + 16384, F32, [KC, BLK])
        self.RT = [self.mk(a + 4096, F32, [BLK]), self.mk(a + 6144, F32, [BLK])]
        self.UT = self.mk(b, F32, [KC, BLK + 2])
        self.YT_conv = self.mk(b, F32, [KC, BLK])
        c0 = b + 16640
        self.CSB = [self.mk(c0, F32, [BLK]), self.mk(c0 + 2048, F32, [BLK])]
        self.ACC = [self.mk(c0 + 4096, F32, [BLK]), self.mk(c0 + 6144, F32, [BLK])]
        self.GT = self.mk(c0 + 8192, BF16, [KC, BLK])
        self.ULAST = self.mk(c0 + 16384, F32, [KC, 2])
        self.CSM = self.mk(c0 + 16384 + 256, F32, [16])
        self.HT2 = self.mk(c0 + 16384 + 512, BF16, [KC, 2])
        self.QT = self.mk(b, BF16, [NH, BLK])
        self.KT = self.mk(b + 4096, BF16, [NH, BLK])
        self.VAUG = self.mk(b + 8192, BF16, [4, NH, VA])
        self.VAUG2 = self.mk(b + 8192, BF16, [16, VA])
        self.YT_m = self.mk(b, F32, [KC, BLK])
        m0 = b + 16640
        self.GO = self.mk(m0, BF16, [4, D])
        m1 = m0 + 8192
        self.PT = self.mk(m1, BF16, [NH, CH])
        self.QS = self.mk(m1 + 1024, BF16, [NH, CH])
        self.KW = self.mk(m1 + 2048, BF16, [NH, CH])
        self.HG = self.mk(m1 + 3072, BF16, [D])
        self.OTMP = self.mk(m1 + 3072, F32, [256])
        self.JUNK = self.mk(m1 + 5120, BF16, [256])
        self.TOKS = self.mk(m1 + 5632, F32, [64])
        self.SM = self.mk(m1 + 5888, F32, [64])
        self.R3 = self.mk(m1 + 6144, F32, [64])
        p0 = b + 32768
        self.CST = self.mk(p0, F32, [NH, VA])
        self.CB = self.mk(p0 + 4352, BF16, [NH, VA])
        self.CIN = self.mk(m1, F32, [NH, VA])
        end = p0 + 4352 + 2304
        assert m1 + 6400 <= p0
        assert end - self.o_arena <= self.ARENA, (end - self.o_arena, self.ARENA)
        self.GA = self.RSTD
        self.GB = self.TB
        self.GC = self.SQ[0]
        self.GD = self.SQ[1]
        self.NBD = self.SQ[0]
        self.EB = self.SQ[1]
        self.IB = self.RSTD
        self.banks = [View(PS[:, i * 512:(i + 1) * 512], "ps", i * 2048, [512], [1], 4) for i in range(8)]
        self.banks_b = [View(PS[:, i * 512:(i + 1) * 512].bitcast(BF16), "ps", i * 2048, [1024], [1], 2)
                        for i in range(8)]

    ARENA = 16384 + 32768 + 4352 + 2304

    def mk(self, off, dt, fshape):
        esz = 4 if dt in (F32, F32R) else 2
        n = int(np.prod(fshape))
        nb = n * esz
        n4 = (nb + 3) // 4
        assert off % 4 == 0
        ap = self.S[:, off // 4: off // 4 + n4]
        if esz == 2:
            ap = ap.bitcast(BF16)[:, :n]
        if len(fshape) == 2:
            ap = ap.rearrange("p (a b) -> p a b", a=fshape[0])
        elif len(fshape) == 3:
            ap = ap.rearrange("p (a b c) -> p a b c", a=fshape[0], b=fshape[1])
        strides = []
        s = 1
        for d_ in reversed(fshape):
            strides.append(s)
            s *= d_
        return View(ap, "sb", off, fshape, list(reversed(strides)), esz)

    def bank(self):
        while True:
            self.bank_rr = (self.bank_rr + 1) % 8
            if self.bank_rr != getattr(self, "stats_bank", None):
                return self.bank_rr

    def stats_push(self, j, src):
        if j == 0:
            self.stats_bank = None
            self.stats_bank = self.bank()
            self.stats_pend = None
        self.stats_flush()
        sq = self.SQB[j % 2]
        self.act(sq, src, AF.Square)
        self.stats_pend = (j, sq)

    def stats_flush(self):
        if self.stats_pend is not None:
            j, sq = self.stats_pend
            self.mm(self.banks[self.stats_bank], self.onesb, sq, start=(j == 0), stop=(j == KC - 1))
            self.stats_pend = None

    def mm(self, out, lhsT, rhs, start=True, stop=True, sig=None):
        return self.P.add("pe", lambda e: e.matmul(out.ap, lhsT.ap, rhs.ap, start=start, stop=stop,
                                                   skip_group_check=True),
                          reads=[lhsT, rhs], writes=[out], force_signal=sig)

    def tr(self, out, in_, ident):
        return self.P.add("pe", lambda e: e.transpose(out.ap, in_.ap, ident.ap),
                          reads=[in_, ident], writes=[out])

    def act(self, out, in_, func, bias=None, scale=None, accum=None):
        reads = [in_]
        kw = {}
        if bias is not None:
            if isinstance(bias, View):
                reads.append(bias)
                kw["bias"] = bias.ap
            else:
                kw["bias"] = float(bias)
        if scale is not None:
            if isinstance(scale, View):
                reads.append(scale)
                kw["scale"] = scale.ap
            else:
                kw["scale"] = float(scale)
        writes = [out]
        if accum is not None:
            writes.append(accum)
            kw["accum_out"] = accum.ap
        return self.P.add("act", lambda e: e.activation(out.ap, in_.ap, func, **kw), reads=reads, writes=writes)

    def _eng(self, name):
        return name

    def tt(self, eng, out, in0, in1, op):
        return self.P.add(eng, lambda e: e.tensor_tensor(out.ap, in0.ap, in1.ap, op), reads=[in0, in1], writes=[out])

    def ts(self, eng, out, in0, s1, s2, op0, op1=None):
        reads = [in0]
        a1 = s1.ap if isinstance(s1, View) else s1
        a2 = s2.ap if isinstance(s2, View) else s2
        if isinstance(s1, View):
            reads.append(s1)
        if isinstance(s2, View):
            reads.append(s2)
        if op1 is None:
            return self.P.add(eng, lambda e: e.tensor_scalar(out.ap, in0.ap, a1, None, op0), reads=reads, writes=[out])
        return self.P.add(eng, lambda e: e.tensor_scalar(out.ap, in0.ap, a1, a2, op0, op1), reads=reads, writes=[out])

    def stt(self, out, in0, scalar, in1, op0, op1):
        reads = [in0, in1]
        a = scalar.ap if isinstance(scalar, View) else scalar
        if isinstance(scalar, View):
            reads.append(scalar)
        return self.P.add("dve", lambda e: e.scalar_tensor_tensor(out.ap, in0.ap, a, in1.ap, op0, op1),
                          reads=reads, writes=[out])

    def cp_(self, eng, out, in_):
        if eng == "act":
            return self.act(out, in_, AF.Copy)
        return self.P.add(eng, lambda e: e.tensor_copy(out.ap, in_.ap), reads=[in_], writes=[out])

    def recip(self, out, in_):
        return self.P.add("dve", lambda e: e.reciprocal(out.ap, in_.ap), reads=[in_], writes=[out])

    def scan(self, out, d0, d1, initial, op0, op1):
        reads = [d0, d1]
        a = initial.ap if isinstance(initial, View) else initial
        if isinstance(initial, View):
            reads.append(initial)
        return self.P.add("dve", lambda e: e.tensor_tensor_scan(out.ap, d0.ap, d1.ap, a, op0, op1),
                          reads=reads, writes=[out])

    def memset(self, eng, out, val):
        return self.P.add(eng, lambda e: e.memset(out.ap, val), writes=[out])

    def dma(self, q, out_ap, in_ap, sem, reads=(), writes=(), deps=(), slow=False):
        if slow:
            fn = lambda e: e.dma_start(out=out_ap, in_=in_ap, allow_slow_non_contiguous=True)
        else:
            fn = lambda e: e.dma_start(out=out_ap, in_=in_ap)
        return self.P.add(q, fn, reads=reads, writes=writes, sig=("dma", sem, 16), deps=deps)

    def step(self, tile, fn, tag=None):
        self.steps.append((tile, fn, getattr(self, "tagpfx", "") + (tag or getattr(fn, "__name__", "?"))))

    def run_steps(self):
        tiles = [(i, t) for i, (t, _, _) in enumerate(self.steps) if t is not None]
        issued = 0
        self.wviews = {}
        ti = 0
        for i, (t, fn, tag) in enumerate(self.steps):
            self.P.cur_tag = tag
            if t is not None:
                while issued < len(tiles) and issued < ti + NB:
                    si, tid = tiles[issued]
                    slot = issued % NB
                    deps = [self.cv_dep[tid]]
                    self.dma("sp", self.S[:, self.o_wb[slot] // 4: self.o_wb[slot] // 4 + TILE_EL // 2].bitcast(BF16),
                             self.wsc[tid], "wb%d" % slot, writes=[self.wb_proj[slot]], deps=deps)
                    self.wviews[si] = slot
                    issued += 1
                slot = self.wviews[i]
                ti += 1
                fn(self.wb_proj[slot], self.wb_down[slot])
            else:
                fn()

    def rms_stats(self, srcs, N, rstd, tb):
        bk = self.banks[self.bank()]
        for kc in range(KC):
            sq = self.SQB[kc % 2][0:N]
            self.act(sq, srcs[kc], AF.Square)
            self.mm(bk[0:N], self.onesb, sq, start=(kc == 0), stop=(kc == KC - 1))
        self.act(rstd[0:N], bk[0:N], AF.Ln, bias=self.eps_col, scale=1.0 / D)
        self.act(rstd[0:N], rstd[0:N], AF.Exp, scale=-0.5)

    def gcol(self, l, j, kc):
        c = CP_G + (l * 4 + j) * 8 + kc
        return self.cp[c:c + 1]

    def pre_norm(self, l, j, t0):
        srcs = [self.xT[kc, t0:t0 + BLK] for kc in range(KC)]
        self.rms_stats(srcs, BLK, self.RSTD, self.TB)
        for kc in range(KC):
            self.stt(self.HT[kc], srcs[kc], self.gcol(l, j, kc), self.RSTD, ALU.mult, ALU.mult)

    def post_norm_residual(self, l, j, t0, YT, fuse_pre=None, unnorm_in=False):
        self.stats_flush()
        bk = self.banks[self.stats_bank]
        self.stats_bank = None
        if unnorm_in:
            self.stt(self.RSTD, self.TB, EPS * D, bk, ALU.mult, ALU.add)
            self.act(self.RSTD, self.RSTD, AF.Ln, scale=1.0 / D)
        else:
            self.act(self.RSTD, bk, AF.Ln, bias=self.eps_col, scale=1.0 / D)
        self.act(self.RSTD, self.RSTD, AF.Exp, scale=-0.5)
        if fuse_pre is not None:
            bk2 = self.banks[self.bank()]
        for kc in range(KC):
            self.stt(YT[kc], YT[kc], self.gcol(l, j, kc), self.RSTD, ALU.mult, ALU.mult)
            xs = self.xT[kc, t0:t0 + BLK]
            self.tt("dve", xs, xs, YT[kc], ALU.add)
            if fuse_pre is not None:
                self.act(self.HT[kc], xs, AF.Copy, scale=self.gcol(l, fuse_pre, kc))
                sq = self.SQB[kc % 2]
                self.act(sq, xs, AF.Square)
                self.mm(bk2, self.onesb, sq, start=(kc == 0), stop=(kc == KC - 1))
        if fuse_pre is not None:
            self.act(self.TB, bk2, AF.Identity, bias=self.eps_col, scale=1.0 / D)
            self.tt("dve", self.TB, self.TB, self.TB, ALU.mult)

    def mlp_block(self, l, blk, prefetch=None):
        t0 = blk * BLK
        base = l * TILES_PER_LAYER
        for hh in range(2):
            for w in range(8):
                def up(wp, wd, w=w):
                    for cc in range(2):
                        fc = w * 2 + cc
                        bk = self.banks[self.bank()]
                        for kc in range(KC):
                            self.mm(bk, wp[kc, cc * 128:(cc + 1) * 128], self.HT[kc], start=(kc == 0), stop=(kc == KC - 1))
                        rt = self.RT[fc % 2]
                        self.act(rt, bk, AF.Relu)
                        self.tt("dve", self.ACTT[fc], rt, rt, ALU.mult)
                self.step(base + 16 + hh * 8 + w, up)
            if hh == 1 and prefetch is not None:
                self.step(None, prefetch, tag="mix_prenorm")
            for j in range(8):
                def down(wp, wd, j=j, hh=hh):
                    bk = self.banks[self.bank()]
                    for fc in range(16):
                        self.mm(bk, wd[fc], self.ACTT[fc], start=(fc == 0), stop=(fc == 15))
                    if hh == 0:
                        self.act(self.YT_mlp[j], bk, AF.Copy)
                    else:
                        self.tt("dve", self.YT_mlp[j], bk, self.YT_mlp[j], ALU.add)
                        self.stats_push(j, self.YT_mlp[j])
                self.step(base + 32 + hh * 8 + j, down)
        self.step(None, lambda: self.post_norm_residual(l, 3, t0, self.YT_mlp, unnorm_in=True), tag="mlp_postnorm")

    def wc(self, lc, j, kc):
        c = CP_WC + (lc * 3 + j) * 8 + kc
        return self.cp[c:c + 1]

    def conv_layer(self, l):
        self.tagpfx = "C."
        lc = l // 2
        base = l * TILES_PER_LAYER
        TOK = self.TOK
        def halo_norm():
            srcs = [self.xT[kc, TOK - 2:TOK] for kc in range(KC)]
            self.rms_stats(srcs, 2, self.RSTD, self.TB)
            for kc in range(KC):
                self.stt(self.HT2[kc], srcs[kc], self.gcol(l, 0, kc), self.RSTD[0:2], ALU.mult, ALU.mult)
        self.step(None, halo_norm)
        hb = {}
        for w in range(8):
            def halo_proj(wp, wd, w=w):
                if w == 0:
                    hb["bk"] = self.banks[self.bank()]
                bk = hb["bk"]
                for cc in range(2):
                    col = (w * 2 + cc) * 2
                    for kc in range(KC):
                        self.mm(bk[col:col + 2], wp[kc, cc * 128:(cc + 1) * 128], self.HT2[kc],
                                start=(kc == 0), stop=(kc == KC - 1))
            self.step(base + 4 + w, halo_proj)

        def halo_finish():
            bk = hb["bk"]
            self.act(self.CSM, bk[0:16], AF.Copy)
            ul = self.ULAST
            self.tt("dve", ul.w(ul.ap.rearrange("p a b -> p (a b)")), self.CSM, bk[16:32], ALU.mult)
            nc = self.nc
            bnc, gth = self.cbounce[lc], self.cgath[lc]
            d1 = self.dma("sp", bnc[:, :], ul.ap.rearrange("p a b -> p (a b)"), "xs", reads=[ul])
            hb["cc"] = self.P.add("pool", lambda e: e.collective_compute(
                "AllGather", ALU.bypass, replica_groups=[[0, 1], [2, 3], [4, 5], [6, 7]],
                ins=[bnc], outs=[gth]), sig=("dma", "cc", 1), deps=[d1])
            self.convert_next(l)
        self.step(None, halo_finish)

        def halo_recv():
            gth = self.cgath[lc]
            up = self.uprev
            self.dma("sp", up.ap.rearrange("p a b -> p (a b)"), gth[0:128, :], "xr", writes=[up], deps=[hb["cc"]])
            self.ts("dve", up, up, self.flag, None, ALU.mult)

        for blk in range(self.NBLK):
            t0 = blk * BLK
            if blk == 0 or not PREFETCH:
                self.step(None, lambda t0=t0: self.pre_norm(l, 0, t0), tag="mix_prenorm")
            st = {}
            for w in range(4):
                def cproj(wp, wd, w=w):
                    for cc in range(2):
                        bk = self.banks[self.bank()]
                        for kc in range(KC):
                            self.mm(bk, wp[kc, cc * 128:(cc + 1) * 128], self.HT[kc], start=(kc == 0), stop=(kc == KC - 1))
                        self.act(self.CSB[cc], bk, AF.Copy)
                self.step(base + 4 + w, cproj)

                def xproj(wp, wd, w=w):
                    for cc in range(2):
                        j = w * 2 + cc
                        bk = self.banks[self.bank()]
                        for kc in range(KC):
                            self.mm(bk, wp[kc, cc * 128:(cc + 1) * 128], self.HT[kc], start=(kc == 0), stop=(kc == KC - 1))
                        self.tt("dve", self.UT[j, 2:BLK + 2], bk, self.CSB[cc], ALU.mult)
                self.step(base + 8 + w, xproj)
            if blk == 0:
                self.step(None, halo_recv)
            for w in range(4):
                def bproj(wp, wd, w=w):
                    if w == 0:
                        self.cp_("dve", self.UT[:, 0:2], self.uprev)
                    for cc in range(2):
                        j = w * 2 + cc
                        bk = self.banks[self.bank()]
                        for kc in range(KC):
                            self.mm(bk, wp[kc, cc * 128:(cc + 1) * 128], self.HT[kc], start=(kc == 0), stop=(kc == KC - 1))
                        acc = self.ACC[cc]
                        self.ts("dve", acc, self.UT[j, 2:BLK + 2], self.wc(lc, 2, j), None, ALU.mult)
                        self.stt(acc, self.UT[j, 1:BLK + 1], self.wc(lc, 1, j), acc, ALU.mult, ALU.add)
                        self.stt(acc, self.UT[j, 0:BLK], self.wc(lc, 0, j), acc, ALU.mult, ALU.add)
                        self.tt("dve", self.GT[j], bk, acc, ALU.mult)
                    if w == 3:
                        self.cp_("dve", self.uprev, self.UT[:, BLK:BLK + 2])
                self.step(base + w, bproj)
            for w in range(4):
                def oproj(wp, wd, w=w):
                    for cc in range(2):
                        j = w * 2 + cc
                        bk = self.banks[self.bank()]
                        for kc in range(KC):
                            self.mm(bk, wp[kc, cc * 128:(cc + 1) * 128], self.GT[kc], start=(kc == 0), stop=(kc == KC - 1))
                        self.act(self.YT_conv[j], bk, AF.Copy)
                        self.stats_push(j, self.YT_conv[j])
                self.step(base + 12 + w, oproj)
            self.step(None, lambda t0=t0: self.post_norm_residual(l, 1, t0, self.YT_conv, fuse_pre=2), tag="mix_postnorm")
            nxt = (lambda t1=t0 + BLK: self.pre_norm(l, 0, t1)) if (PREFETCH and blk + 1 < self.NBLK) else None
            self.mlp_block(l, blk, prefetch=nxt)

    def bgate(self, lm, which):
        c = CP_BG + lm * 2 + which
        return self.cp[c:c + 1].p(0, 4)

    def gate_rows(self, lm, blk, gi_bank, gf_bank, main):
        self.gate_rows_a(lm, blk, gi_bank, gf_bank, main)
        self.gate_rows_b(lm, blk, main)

    def gate_rows_a(self, lm, blk, gi_bank, gf_bank, main):
        A, B, C_, Dd = (self.GA.p(0, 4), self.GB.p(0, 4), self.GC.p(0, 4), self.GD.p(0, 4))
        self.ts("dve", A, gi_bank.p(0, 4), self.bgate(lm, 0), None, ALU.add)
        self.ts("dve", B, gf_bank.p(0, 4), self.bgate(lm, 1), None, ALU.add)
        self.act(A, A, AF.Tanh, scale=1.0 / CAP)
        self.act(B, B, AF.Tanh, scale=1.0 / CAP)
        self.act(B, B, AF.Exp, scale=-CAP)
        self.act(B, B, AF.Ln, bias=self.one_col.p(0, 4))
        ones_row = self.ones.p(0, 4)
        for c in range(4):
            cs = slice(c * CH, (c + 1) * CH)
            self.scan(C_[cs], ones_row, B[cs], 0.0, ALU.mult, ALU.subtract)
        self.stt(A, A, CAP, C_, ALU.mult, ALU.subtract)
        for c in range(4):
            cs = slice(c * CH, (c + 1) * CH)
            gc = blk * 4 + c
            self.scan(B[cs], A[cs], A[cs], self.mpv[gc:gc + 1].p(0, 4), ALU.max, ALU.max)
            self.tt("dve", self.mpv[gc + 1:gc + 2].p(0, 4), C_[c * CH + CH - 1:c * CH + CH], B[c * CH + CH - 1:c * CH + CH], ALU.add)
        A3 = A.ap.rearrange("p (c t) -> p c t", c=4)
        B3 = B.ap.rearrange("p (c t) -> p c t", c=4)
        D3 = Dd.ap.rearrange("p (c t) -> p c t", c=4)
        self.tt("dve", Dd.w(D3), A.w(A3), B.w(B3[:, :, CH - 1:CH].to_broadcast([4, 4, CH])), ALU.subtract)
        self.act(Dd, Dd, AF.Exp)
        if main:
            self.tt("dve", C_, C_, B, ALU.add)
            self.act(C_, C_, AF.Exp, scale=-1.0)

    def gate_rows_b(self, lm, blk, main):
        A, B, C_, Dd = (self.GA.p(0, 4), self.GB.p(0, 4), self.GC.p(0, 4), self.GD.p(0, 4))
        B3 = B.ap.rearrange("p (c t) -> p c t", c=4)
        mb = self.banks[7]
        qs = [(A, 0), (Dd, 1)] + ([(C_, 2)] if main else [])
        for c in range(4):
            for row, qi in qs:
                col = (c * 3 + qi) * 4
                self.mm(mb[col:col + 4], row[c * CH:(c + 1) * CH], self.diag4, start=True, stop=True)
        r3 = self.R3.p(0, 4)
        g0 = blk * 4
        self.tt("dve", r3[0:4], self.mpv[g0:g0 + 4].p(0, 4), B.w(B3[:, :, CH - 1]), ALU.subtract)
        r16 = r3[4:20]
        self.tt("dve", r16.w(r16.ap.rearrange("p (c h) -> p c h", c=4)),
                r3[0:4].w(r3[0:4].ap.unsqueeze(2).to_broadcast([4, 4, 4])),
                self.diag4.w(self.diag4.ap.unsqueeze(1).to_broadcast([4, 4, 4])), ALU.mult)
        self.mm(mb[48:64], self.ones.p(0, 4), r16, start=True, stop=True)
        self.cp_("dve", self.TOKS[0:48], mb[0:48])
        self.act(self.TOKS[48:64], mb[48:64], AF.Exp)

    def tok(self, c, qi, h):
        col = (c * 3 + qi) * 4 + h
        return self.TOKS[col:col + 1]

    def state_update(self, c, main):
        self.state_kw(c)
        self.state_kv(c, main)

    def state_kw(self, c):
        kb = self.banks_b[6]
        for h in range(NH):
            self.tr(kb[h * CH:(h + 1) * CH], self.KT[h, c * CH:(c + 1) * CH], self.identb)
        for h in range(NH):
            if h % 2 == 0:
                self.act(self.KW[h], kb[h * CH:(h + 1) * CH], AF.Copy, scale=self.tok(c, 1, h))
            else:
                self.ts("dve", self.KW[h], kb[h * CH:(h + 1) * CH], self.tok(c, 1, h), None, ALU.mult)

    def state_kv(self, c, main):
        for h in range(NH):
            nb = self.banks[h]
            self.mm(nb[0:VA], self.KW[h], self.VAUG[c, h], start=True, stop=True)
        for h in range(NH):
            nb = self.banks[h]
            il = self.TOKS[48 + c * 4 + h:48 + c * 4 + h + 1]
            self.stt(self.CST[h], self.CST[h], il, nb[0:VA], ALU.mult, ALU.add)
        if main:
            self.cp_("act", self.CB, self.CST)

    def proj_feat(self, wp, dst_fn, evac):
        for cc in range(2):
            bk = self.banks[self.bank()]
            for kc in range(KC):
                self.mm(bk, wp[kc, cc * 128:(cc + 1) * 128], self.HT[kc], start=(kc == 0), stop=(kc == KC - 1))
            evac(cc, bk)

    def proj_tok(self, wp, evac):
        for c in range(4):
            bi = self.bank()
            bk = self.banks[bi]
            half = bk[0:256]
            for kc in range(KC):
                self.mm(half, self.HT[kc, c * CH:(c + 1) * CH], wp[kc], start=(kc == 0), stop=(kc == KC - 1))
            evac(c, half)

    def gates_proj(self, st, lm):
        self.wg = self.wgs[lm]
        gi = self.banks[self.bank()]
        gf = self.banks[self.bank()]
        for kc in range(KC):
            self.mm(gi.p(0, 4), self.wg[kc, 0:4], self.HT[kc], start=(kc == 0), stop=(kc == KC - 1))
        for kc in range(KC):
            self.mm(gf.p(0, 4), self.wg[kc, 4:8], self.HT[kc], start=(kc == 0), stop=(kc == KC - 1))
        st["gi"], st["gf"] = gi, gf

    def mlstm_layer(self, l):
        lm = l // 2
        base = l * TILES_PER_LAYER
        nc = self.nc
        w_in = self.w_in_m

        def layer_init():
            self.dma("sp", self.ghn.ap, self.ghn_d[lm], "ghn", writes=[self.ghn])
            self.ts("dve", self.ghn, self.ghn, 0.5, None, ALU.mult)
            self.memset("dve", self.CST, 0.0)
            self.memset("dve", self.mpv.p(0, 4), 0.0)
        self.step(None, layer_init)

        self.tagpfx = "P."
        for blk in range(self.NBLK):
            t0 = blk * BLK
            st = {}
            if blk == 0 or not PREFETCH:
                self.step(None, lambda t0=t0: self.pre_norm(l, 0, t0), tag="mix_prenorm")
            for w in range(2):
                def kproj(wp, wd, w=w):
                    def ev(cc, bk):
                        self.act(self.KT[w * 2 + cc], bk, AF.Copy, scale=128.0 ** -0.5)
                    self.proj_feat(wp, None, ev)
                self.step(base + 2 + w, kproj)
            for w in range(4):
                def vproj(wp, wd, w=w, st=st, blk=blk):
                    if w == 0:
                        self.memset("dve", self.VAUG2[:, 256:257], 1.0)
                        self.gates_proj(st, lm)
                        self.gate_rows_a(lm, blk, st["gi"], st["gf"], main=False)
                    def ev(c, half):
                        self.act(self.VAUG[c, w, 0:256], half, AF.Copy)
                    self.proj_tok(wp, ev)
                self.step(base + 4 + w, vproj)

            def pre_rows(blk=blk):
                self.gate_rows_b(lm, blk, main=False)
            self.step(None, pre_rows, tag="pre_chunks")
            if PREFETCH and blk + 1 < self.NBLK:
                self.step(None, lambda t1=t0 + BLK: self.pre_norm(l, 0, t1), tag="mix_prenorm")

            def pre_chunks():
                for c in range(4):
                    self.state_update(c, main=False)
            self.step(None, pre_chunks)

        xs = {}

        def exchange_send():
            bnc, gth = self.mbounce[lm], self.mgath[lm]
            cflat = self.CST.ap.rearrange("p h v -> p (h v)")
            d1 = self.dma("sp", bnc[:, 0:NH * VA], cflat, "xs", reads=[self.CST])
            gl = self.NBLK * 4
            d2 = self.dma("sp", bnc[0:4, NH * VA:NH * VA + 1], self.mpv[gl:gl + 1].p(0, 4).ap, "xs",
                          reads=[self.mpv[gl:gl + 1]], slow=True)
            xs["cc"] = self.P.add("pool", lambda e: e.collective_compute(
                "AllGather", ALU.bypass, replica_groups=[[0, 1], [2, 3], [4, 5], [6, 7]],
                ins=[bnc], outs=[gth]), sig=("dma", "cc", 1), deps=[d1, d2])
            self.convert_next(l)
        self.step(None, exchange_send)
        self.tagpfx = "M."

        def exchange_recv():
            gth = self.mgath[lm]
            cc = xs["cc"]
            self.dma("sp", self.CIN.ap.rearrange("p h v -> p (h v)"), gth[0:128, 0:NH * VA], "xr",
                     writes=[self.CIN], deps=[cc])
            self.dma("sp", self.min_[0:1].p(0, 4).ap, gth[0:4, NH * VA:NH * VA + 1], "xr2",
                     writes=[self.min_[0:1]], deps=[cc], slow=True)
            self.ts("dve", self.CST, self.CIN, self.flag, None, ALU.mult)
            self.cp_("act", self.CB, self.CST)
            self.ts("dve", self.mpv[0:1].p(0, 4), self.min_[0:1].p(0, 4), self.flag.p(0, 4), None, ALU.mult)

        for blk in range(self.NBLK):
            t0 = blk * BLK
            st = {}
            if blk == 0 or not PREFETCH:
                self.step(None, lambda t0=t0: self.pre_norm(l, 0, t0), tag="mix_prenorm")
            for w in range(2):
                def qproj(wp, wd, w=w):
                    def ev(cc, bk):
                        self.act(self.QT[w * 2 + cc], bk, AF.Copy)
                    self.proj_feat(wp, None, ev)
                self.step(base + w, qproj)
            for w in range(2):
                def kproj(wp, wd, w=w):
                    def ev(cc, bk):
                        self.act(self.KT[w * 2 + cc], bk, AF.Copy, scale=128.0 ** -0.5)
                    self.proj_feat(wp, None, ev)
                self.step(base + 2 + w, kproj)
            for w in range(4):
                def vproj(wp, wd, w=w, st=st, blk=blk):
                    if w == 0:
                        self.memset("dve", self.VAUG2[:, 256:257], 1.0)
                    def ev(c, half):
                        self.act(self.VAUG[c, w, 0:256], half, AF.Copy)
                    self.proj_tok(wp, ev)
                self.step(base + 4 + w, vproj)
            for w in range(4):
                def oproj(wp, wd, w=w, st=st, blk=blk):
                    if w == 0 and blk > 0:
                        self.gates_proj(st, lm)
                        self.gate_rows_a(lm, blk, st["gi"], st["gf"], main=True)
                    if w == 2 and blk > 0:
                        self.gate_rows_b(lm, blk, main=True)
                        self.chunk_s1(blk, 0)

                    def ev(c, half):
                        tmp = self.OTMP
                        self.act(tmp, half, AF.Tanh, scale=0.5)
                        self.stt(self.GO[c, w * 256:(w + 1) * 256], tmp, 1.0, self.ghn[w * 256:(w + 1) * 256],
                                 ALU.add, ALU.mult)
                    self.proj_tok(wp, ev)
                self.step(base + 8 + w, oproj)

            if blk == 0:
                self.step(None, exchange_recv)

            def chunks(blk=blk, st=st):
                if blk == 0:
                    self.gates_proj(st, lm)
                    self.gate_rows_a(lm, blk, st["gi"], st["gf"], main=True)
                    self.gate_rows_b(lm, blk, main=True)
                    self.chunk_s1(blk, 0)
                for c in range(4):
                    self.chunk_s2a(blk, c)
                    if c < 3:
                        self.chunk_s1(blk, c + 1)
                    self.state_kw(c)
                    self.chunk_s2b(blk, c)
                    if c < 3:
                        self.state_kv(c, main=True)
                        self.chunk_s2c(blk, c)
                    else:
                        self.chunk_s2c(blk, c)
                        self.state_kv(c, main=True)
            self.step(None, chunks)
            for w in range(4):
                def outproj(wp, wd, w=w):
                    for cc in range(2):
                        j = w * 2 + cc
                        bk = self.banks[self.bank()]
                        for kc in range(KC):
                            self.mm(bk, wp[kc, cc * 128:(cc + 1) * 128], self.HT[kc], start=(kc == 0), stop=(kc == KC - 1))
                        self.act(self.YT_m[j], bk, AF.Copy)
                        self.stats_push(j, self.YT_m[j])
                self.step(base + 12 + w, outproj)
            self.step(None, lambda t0=t0: self.post_norm_residual(l, 1, t0, self.YT_m, fuse_pre=2), tag="mix_postnorm")
            nxt = (lambda t1=t0 + BLK: self.pre_norm(l, 0, t1)) if (PREFETCH and blk + 1 < self.NBLK) else None
            self.mlp_block(l, blk, prefetch=nxt)

    def chunk_s1(self, blk, c):
        gc = blk * 4 + c
        cs = slice(c * CH, (c + 1) * CH)
        Mrow = self.GB.p(0, 4)
        nbd = self.NBD.p(0, 4)
        nbd3 = nbd.ap.rearrange("p (h t) -> p h t", h=4)
        self.tt("dve", nbd.w(nbd3), Mrow[cs].w(Mrow[cs].ap.unsqueeze(1).to_broadcast([4, 4, CH])),
                self.negdiag4.w(self.negdiag4.ap.unsqueeze(2).to_broadcast([4, 4, CH])), ALU.mult)
        BC, BC2, SS = self.banks[4], self.banks[5], self.banks[6]
        ones4 = self.ones.p(0, 4)
        self.mm(BC, ones4, nbd, start=True, stop=True)
        self.stt(nbd.w(nbd3), self.diag4.w(self.diag4.ap.unsqueeze(2).to_broadcast([4, 4, CH])),
                 self.mpv[gc:gc + 1].p(0, 4), nbd.w(nbd3), ALU.mult, ALU.add)
        self.mm(BC2, ones4, nbd, start=True, stop=True)
        for h in range(NH):
            self.mm(SS[h * CH:(h + 1) * CH], self.KT[h, cs], self.QT[h, cs], start=True, stop=True)
        E = self.EB
        for h in range(NH):
            self.stt(E[h * CH:(h + 1) * CH], BC[h * CH:(h + 1) * CH], self.tok(c, 0, h), self.maskneg, ALU.add, ALU.min)
        self.act(E, E, AF.Exp)
        self.tt("dve", self.PT.w(self.PT.ap.rearrange("p h t -> p (h t)")), SS, E, ALU.mult)
        self.act(self.IB, BC2, AF.Exp)
        ib3 = self.IB.ap.rearrange("p (h t) -> p h t", h=4)
        self.tt("dve", self.QS, self.QT[:, cs], self.IB.w(ib3), ALU.mult)

    def chunk_s2a(self, blk, c):
        for h in range(NH):
            self.mm(self.banks[h][0:VA], self.PT[h], self.VAUG[c, h], start=True, stop=False)
        for h in range(NH):
            self.mm(self.banks[h][0:VA], self.QS[h], self.CB[h], start=False, stop=True)

    def chunk_s2b(self, blk, c):
        cs = slice(c * CH, (c + 1) * CH)
        sm = self.SM
        dmax, r, ssq, t4, rs2, sc = (sm[0:4], sm[4:8], sm[8:12], sm[12:16], sm[16:20], sm[20:24])
        den_ap = self.PS[:, 0:4 * 512].rearrange("p (b c) -> p b c", b=4)[:, :, 256]
        den = View(den_ap, "ps", 0, [4 * 512], [1], 4)
        fl = self.TOKS[(c * 3 + 2) * 4:(c * 3 + 2) * 4 + 4]
        self.act(dmax, den, AF.Abs)
        for h in range(NH):
            self.act(self.JUNK, self.banks[h][0:256], AF.Square, accum=ssq[h:h + 1])
        self.tt("dve", dmax, dmax, fl, ALU.max)
        self.recip(r, dmax)
        self.tt("dve", t4, r, r, ALU.mult)
        self.tt("dve", t4, t4, ssq, ALU.mult)
        self.act(t4, t4, AF.Ln, bias=self.eps_col, scale=1.0 / 256)
        self.act(rs2, t4, AF.Exp, scale=-0.5)
        self.tt("dve", sc, r, rs2, ALU.mult)
        for h in range(NH):
            self.stt(self.HG[h * 256:(h + 1) * 256], self.banks[h][0:256], sc[h:h + 1],
                     self.GO[c, h * 256:(h + 1) * 256], ALU.mult, ALU.mult)

    def chunk_s2c(self, blk, c):
        cs = slice(c * CH, (c + 1) * CH)
        tb_ = self.banks_b[7]
        for j in range(KC):
            self.tr(tb_[j * CH:(j + 1) * CH], self.HG[j * CH:(j + 1) * CH], self.identb)
        hgt = self.HT[:, cs]
        self.act(hgt, tb_.w(tb_.ap.rearrange("p (j t) -> p j t", j=KC)), AF.Copy)

    def build(self):
        nc = self.nc
        TOK = self.TOK
        dt = nc.dram_tensor
        self.xT_d = dt("xT", [D, TOK], F32, kind="ExternalInput").ap()
        self.cp_d = dt("cpack", [128, CP_COLS], F32, kind="ExternalInput").ap()
        self.ghn_d = dt("ghn", [2, 128, D], F32, kind="ExternalInput").ap()
        self.w_in_m = dt("w_in_mlstm", [2, D, 3080], F32, kind="ExternalInput").ap()
        self.w_out_m = dt("w_out_mlstm", [2, D, D], F32, kind="ExternalInput").ap()
        self.w_in_c = dt("w_in_conv", [2, D, 3072], F32, kind="ExternalInput").ap()
        self.w_out_c = dt("w_out_conv", [2, D, D], F32, kind="ExternalInput").ap()
        self.w_up = dt("w_mlp_up", [4, D, DFF], F32, kind="ExternalInput").ap()
        self.w_dn = dt("w_mlp_down", [4, DFF, D], F32, kind="ExternalInput").ap()
        self.yT_d = dt("yT", [D, TOK], F32, kind="ExternalOutput").ap()
        self.wsc = dt("wsc", [4 * TILES_PER_LAYER, 128, TILE_EL], BF16, kind="Internal").ap()
        self.mbounce = [dt("mb%d" % i, [128, 1040], F32, kind="Internal").ap() for i in range(2)]
        self.mgath = [dt("mg%d" % i, [256, 1040], F32, kind="Internal").ap() for i in range(2)]
        self.cbounce = [dt("cb%d" % i, [128, 16], F32, kind="Internal").ap() for i in range(2)]
        self.cgath = [dt("cg%d" % i, [256, 16], F32, kind="Internal").ap() for i in range(2)]

        with contextlib.ExitStack() as es:
            tot_probe = KC * TOK * 4
            self.S = None
            dummy_total = self._layout_total()
            S = es.enter_context(nc.sbuf_tensor("S", [128, dummy_total // 4], F32))
            PS = es.enter_context(nc.psum_tensor("PS", [128, 4096], F32))
            self.setup_memory(S, PS)
            assert self.total == dummy_total
            self.eps_col = self.mk(self.o_persist + 768, F32, [1])
            self.one_col = self.mk(self.o_persist + 772, F32, [1])
            self.program()
            self.P.finalize()
            sems = {n: es.enter_context(nc.semaphore(n)) for n in self.P.sem_names()}
            block = es.enter_context(nc.Block())
            P = self.P

            @block.tensor
            def _(e):
                P.emit("pe", e, sems)

            @block.scalar
            def _(e):
                P.emit("act", e, sems)

            @block.vector
            def _(e):
                P.emit("dve", e, sems)

            @block.gpsimd
            def _(e):
                P.emit("pool", e, sems)

            @block.sync
            def _(e):
                P.emit("sp", e, sems)
        return nc

    def _layout_total(self):
        off = 0

        def take(nbytes):
            nonlocal off
            off += (nbytes + CELL - 1) // CELL * CELL
        take(KC * self.TOK * 4)
        take(CP_COLS * 4)
        take(512)
        take(D * 4)
        take(512)
        for _ in range(NB):
            take(TILE_EL * 2)
        take(1024)
        return off + self.ARENA

    def convert_layer(self, l):
        lidx = l // 2
        base = l * TILES_PER_LAYER
        if l % 2 == 0:
            win, wout = self.w_in_m[lidx], self.w_out_m[lidx]
        else:
            win, wout = self.w_in_c[lidx], self.w_out_c[lidx]
        winr = win.rearrange("(kc p) c -> p kc c", p=128)
        woutr = wout.rearrange("(kc p) c -> p kc c", p=128)
        wupr = self.w_up[l].rearrange("(kc p) c -> p kc c", p=128)
        wdnr = self.w_dn[l].rearrange("(fc p) c -> p fc c", p=128)
        first = (l == self.layers[0]) and l % 2 == 0
        order = ([2, 3, 4, 5, 6, 7] + [t for t in range(TILES_PER_LAYER) if t not in (2, 3, 4, 5, 6, 7)]) if first \
            else list(range(TILES_PER_LAYER))
        ga = []
        for n_, t in enumerate(order):
            if t < 12:
                src = winr[:, :, t * 256:(t + 1) * 256]
                dst = self.wsc[base + t].rearrange("p (a b) -> p a b", a=KC)
            elif t < 16:
                src = woutr[:, :, (t - 12) * 256:(t - 11) * 256]
                dst = self.wsc[base + t].rearrange("p (a b) -> p a b", a=KC)
            elif t < 32:
                src = wupr[:, :, (t - 16) * 256:(t - 15) * 256]
                dst = self.wsc[base + t].rearrange("p (a b) -> p a b", a=KC)
            else:
                hh, j = divmod(t - 32, 8)
                src = wdnr[:, hh * 16:(hh + 1) * 16, j * 128:(j + 1) * 128]
                dst = self.wsc[base + t].rearrange("p (a b) -> p a b", a=16)
            early = first and n_ < 6
            d_ = self.dma("pool", dst, src, ("cva%d" if early else "cv%d") % l)
            if early:
                ga.append(base + t)
                for tt_ in ga:
                    self.cv_dep[tt_] = d_
            else:
                for t2 in order[(6 if first else 0):]:
                    self.cv_dep[base + t2] = d_

    def convert_next(self, l):
        i = self.layers.index(l)
        if i + 1 < len(self.layers):
            self.convert_layer(self.layers[i + 1])

    def program(self):
        TOK = self.TOK
        self.dma("sp", self.cp.ap, self.cp_d, "cpl", writes=[self.cp])
        xr = self.xT_d.rearrange("(kc p) t -> p kc t", p=128)
        for kc in range(KC):
            self.dma("sp", self.xT[kc].ap, xr[:, kc, :], "xl%d" % kc, writes=[self.xT[kc]])
        self.cp_("dve", self.identb, self.ident)
        self.cp_("dve", self.onesb, self.ones)
        self.memset("dve", self.eps_col, EPS)
        self.memset("dve", self.one_col, 1.0)
        for lm in range(2):
            if 2 * lm in self.layers:
                self.dma("pool", self.wgs[lm].ap,
                         self.w_in_m[lm].rearrange("(kc p) c -> p kc c", p=128)[:, :, 3072:3080], "wgl%d" % lm,
                         writes=[self.wgs[lm]])
        self.cv_dep = {}
        self.convert_layer(self.layers[0])
        for l in self.layers:
            if l % 2 == 0:
                self.mlstm_layer(l)
            else:
                self.conv_layer(l)
        self.run_steps()
        yr = self.yT_d.rearrange("(kc p) t -> p kc t", p=128)
        outs = []
        for kc in range(KC):
            outs.append(self.dma("sp", yr[:, kc, :], self.xT[kc].ap, "xl%d" % kc, reads=[self.xT[kc]]))
        self.P.add("sp", None, sig=None, deps=outs)


def build_nc(TOK, layers):
    nc = bass.Bass("TRN2", target_bir_lowering=False)
    k = K(nc, TOK, layers)
    k.build()
    return nc, k


def make_cpack(norm_g, w_conv, b_gates, flag):
    cp = np.zeros((128, CP_COLS), np.float32)
    cp[:, CP_IDENT:CP_IDENT + 128] = np.eye(128, dtype=np.float32)
    s = np.arange(128)[:, None]
    t = np.arange(128)[None, :]
    cp[:, CP_MASK:CP_MASK + 128] = np.where(s <= t, 0.0, -30000.0).astype(np.float32)
    cp[:, CP_ONES:CP_ONES + 128] = 1.0
    cp[:, CP_G:CP_G + 128] = norm_g.reshape(4, 4, KC, 128).transpose(3, 0, 1, 2).reshape(128, 128)
    cp[:, CP_WC:CP_WC + 48] = w_conv.reshape(2, 3, KC, 128).transpose(3, 0, 1, 2).reshape(128, 48)
    cp[:, CP_NEGHALF] = -0.5
    cp[0:4, CP_DIAG4:CP_DIAG4 + 4] = np.eye(4, dtype=np.float32)
    cp[0:4, CP_NEGDIAG4:CP_NEGDIAG4 + 4] = -np.eye(4, dtype=np.float32)
    for lm in range(2):
        cp[0:4, CP_BG + lm * 2 + 0] = b_gates[lm, 0:4]
        cp[0:4, CP_BG + lm * 2 + 1] = b_gates[lm, 4:8]
    cp[:, CP_FLAG] = flag
    return cp


_CACHE = {}
LAST = {}


def run(x, norm_g, w_in_mlstm, b_gates_mlstm, g_hnorm, w_out_mlstm, w_in_conv, w_conv, w_out_conv,
        w_mlp_up, w_mlp_down, layers=(0, 1, 2, 3), trace=False):
    B, S, _ = x.shape
    assert B * 2 == NCORES
    TOK = S // 2
    key = (TOK, tuple(layers))
    if key not in _CACHE:
        _CACHE[key] = build_nc(TOK, list(layers))[0]
    nc = _CACHE[key]
    f = lambda a: np.ascontiguousarray(np.asarray(a, dtype=np.float32))
    ghn = np.ascontiguousarray(np.broadcast_to(f(g_hnorm)[:, None, :], (2, 128, D)))
    shared = {
        "ghn": ghn,
        "w_in_mlstm": f(w_in_mlstm), "w_out_mlstm": f(w_out_mlstm),
        "w_in_conv": f(w_in_conv), "w_out_conv": f(w_out_conv),
        "w_mlp_up": f(w_mlp_up), "w_mlp_down": f(w_mlp_down),
    }
    xf = f(x)
    in_maps = []
    for c in range(NCORES):
        b, half = divmod(c, 2)
        m = dict(shared)
        m["xT"] = np.ascontiguousarray(xf[b, half * TOK:(half + 1) * TOK, :].T)
        m["cpack"] = make_cpack(f(norm_g), f(w_conv), f(b_gates_mlstm), float(half))
        in_maps.append(m)
    res = run_bass_kernel_spmd(nc, in_maps, core_ids=list(range(NCORES)), **({"trace": True} if trace else {}))
    LAST["exec_ns"] = getattr(res, "exec_time_ns", None)
    out = np.empty((B, S, D), np.float32)
    for c in range(NCORES):
        b, half = divmod(c, 2)
        out[b, half * TOK:(half + 1) * TOK, :] = res.results[c]["yT"].T
    return out


def kernel(x, norm_g, w_in_mlstm, b_gates_mlstm, g_hnorm, w_out_mlstm, w_in_conv, w_conv, w_out_conv,
           w_mlp_up, w_mlp_down):
    return run(x, norm_g, w_in_mlstm, b_gates_mlstm, g_hnorm, w_out_mlstm, w_in_conv, w_conv, w_out_conv,
               w_mlp_up, w_mlp_down)
```

```python
import contextlib
import numpy as np
import concourse.bass as bass
import concourse.mybir as mybir
from concourse.bass_utils import run_bass_kernel_spmd

F32 = mybir.dt.float32
F32R = mybir.dt.float32r
BF16 = mybir.dt.bfloat16
AF = mybir.ActivationFunctionType
ALU = mybir.AluOpType

D = 1024
KC = 8
NH = 4
DFF = 4096
EPS = 1e-6
CAP = 15.0
NCORES = 8
BLK = 512
CH = 128
TILE_EL = 2048
NB = 4
PREFETCH_LITE = True
PREFETCH = False
TILES_PER_LAYER = 48
VA = 257
CELL = 256


CP_IDENT = 0
CP_MASK = 128
CP_ONES = 256
CP_G = 384
CP_WC = 512
CP_NEGHALF = 560
CP_DIAG4 = 562
CP_NEGDIAG4 = 566
CP_BG = 570
CP_FLAG = 574
CP_COLS = 576


class View:
    __slots__ = ("ap", "space", "base", "fshape", "fstr", "esz")

    def __init__(self, ap, space, base, fshape, fstr, esz):
        self.ap = ap
        self.space = space
        self.base = base
        self.fshape = list(fshape)
        self.fstr = list(fstr)
        self.esz = esz

    @property
    def lo(self):
        return self.base

    @property
    def hi(self):
        return self.base + (sum((n - 1) * s for n, s in zip(self.fshape, self.fstr)) + 1) * self.esz

    def __getitem__(self, idx):
        if not isinstance(idx, tuple):
            idx = (idx,)
        idx = idx + (slice(None),) * (len(self.fshape) - len(idx))
        base = self.base
        fshape, fstr = [], []
        for i, n, s in zip(idx, self.fshape, self.fstr):
            if isinstance(i, int):
                base += i * s * self.esz
            else:
                st, sp, step = i.indices(n)
                assert step == 1
                base += st * s * self.esz
                fshape.append(sp - st)
                fstr.append(s)
        return View(self.ap[(slice(None),) + idx], self.space, base, fshape, fstr, self.esz)

    def p(self, p0, p1):
        return View(self.ap[p0:p1], self.space, self.base, self.fshape, self.fstr, self.esz)

    def w(self, ap):
        return View(ap, self.space, self.base, self.fshape, self.fstr, self.esz)


class Ins:
    __slots__ = ("q", "fn", "raw", "oth", "sig", "epos", "signaled", "ev", "waits", "gidx", "tag")

    def __init__(self, q, fn, sig):
        self.q = q
        self.fn = fn
        self.raw = set()
        self.oth = set()
        self.sig = sig
        self.signaled = False
        self.ev = None
        self.waits = []


class Prog:
    CELLS = {"sb": CELL, "ps": 2048}

    def __init__(self):
        self.all = []
        self.byq = {q: [] for q in ("pe", "act", "dve", "pool", "sp")}
        self.lastw = {"sb": {}, "ps": {}}
        self.readers = {"sb": {}, "ps": {}}
        self.last_sig_pos = {q: -1 for q in self.byq}
        self.cur_tag = "init"

    def _stream(self, ins):
        return ins.q if ins.sig == "eng" or ins.sig is None else ins.sig[1]

    def _cells(self, v):
        cs = self.CELLS[v.space]
        return range(v.lo // cs, (v.hi - 1) // cs + 1)

    def add(self, q, fn, reads=(), writes=(), sig="eng", deps=(), force_signal=None):
        ins = Ins(q, fn, sig)
        ins.epos = len(self.byq[q])
        ins.gidx = len(self.all)
        ins.tag = self.cur_tag
        for v in reads:
            lw, rd = self.lastw[v.space], self.readers[v.space]
            ps_as_write = v.space == "ps"
            for c in self._cells(v):
                w = lw.get(c)
                if w is not None:
                    ins.raw.add(w)
                if ps_as_write:
                    for r in rd.get(c, {}).values():
                        ins.oth.add(r)
                    lw[c] = ins
                    rd[c] = {}
                else:
                    rd.setdefault(c, {})[self._stream(ins)] = ins
        for v in writes:
            lw, rd = self.lastw[v.space], self.readers[v.space]
            for c in self._cells(v):
                w = lw.get(c)
                if w is not None:
                    ins.oth.add(w)
                for r in rd.get(c, {}).values():
                    ins.oth.add(r)
                lw[c] = ins
                rd[c] = {}
        for d in deps:
            ins.raw.add(d)
        ins.raw.discard(ins)
        ins.oth.discard(ins)
        keep = set()
        for d in ins.raw:
            if d.q == ins.q and d.sig == "eng" and ins.sig in ("eng", None) and ins.q == "pe":
                continue
            keep.add(d)
        for d in ins.oth:
            if d.q == ins.q and d.sig == "eng" and ins.sig in ("eng", None):
                continue
            keep.add(d)
        ins.raw = keep
        ins.oth = set()
        if sig == "eng":
            if q != "pe" or force_signal:
                ins.signaled = True
        elif sig is not None:
            ins.signaled = True
        self.all.append(ins)
        self.byq[q].append(ins)
        if ins.signaled and sig == "eng":
            self.last_sig_pos[q] = ins.epos
        for d in ins.raw:
            if d.sig == "eng" and not d.signaled and self.last_sig_pos[d.q] < d.epos:
                last = self.byq[d.q][-1]
                if last.sig != "eng":
                    raise RuntimeError("cannot signal")
                last.signaled = True
                self.last_sig_pos[d.q] = last.epos
        return ins

    def finalize(self):
        self.nextsig = {}
        for q, lst in self.byq.items():
            cnt = 0
            for i in lst:
                if i.sig == "eng" and i.signaled:
                    cnt += 1
                    i.ev = ("E_" + q, cnt)
            nxt = None
            arr = [None] * len(lst)
            for pos in range(len(lst) - 1, -1, -1):
                if lst[pos].sig == "eng" and lst[pos].signaled:
                    nxt = lst[pos]
                arr[pos] = nxt
            self.nextsig[q] = arr
        dmacnt = {}
        for i in self.all:
            if i.sig not in ("eng", None):
                _, sem, amt = i.sig
                dmacnt[sem] = dmacnt.get(sem, 0) + amt
                i.ev = (sem, dmacnt[sem])
        clock = {q: {} for q in self.byq}
        snap = {}
        nwaits = 0
        for i in self.all:
            ck = clock[i.q]
            need = {}
            for d in i.raw:
                if d.sig == "eng":
                    r = self.nextsig[d.q][d.epos]
                    assert r is not None, "unsignaled dependency"
                else:
                    r = d
                s, v = r.ev
                if ck.get(s, 0) >= v:
                    continue
                if need.get(s, (0, None))[0] < v:
                    need[s] = (v, r)
            for s, (v, r) in need.items():
                if ck.get(s, 0) >= v:
                    continue
                i.waits.append((s, v))
                nwaits += 1
                for s2, v2 in snap[r.ev].items():
                    if ck.get(s2, 0) < v2:
                        ck[s2] = v2
            if i.ev is not None:
                sn = dict(ck)
                sn[i.ev[0]] = i.ev[1]
                snap[i.ev] = sn
        self.nwaits = nwaits

    def emit(self, q, e, sems):
        for i in self.byq[q]:
            for s, v in i.waits:
                e.wait_ge(sems[s], v)
            if i.fn is None:
                continue
            bi = i.fn(e)
            if i.ev is not None:
                amt = 1 if i.sig == "eng" else i.sig[2]
                bi.then_inc(sems[i.ev[0]], amt)

    def sem_names(self):
        names = set()
        for i in self.all:
            if i.ev is not None:
                names.add(i.ev[0])
        return sorted(names)


class K:
    def __init__(self, nc, TOK, layers):
        self.nc = nc
        self.TOK = TOK
        self.layers = layers
        self.NBLK = TOK // BLK
        self.P = Prog()
        self.steps = []
        self.bank_rr = 0

    def setup_memory(self, S, PS):
        self.S = S
        self.PS = PS
        off = 0

        def take(nbytes):
            nonlocal off
            o = off
            off += (nbytes + CELL - 1) // CELL * CELL
            return o

        self.o_xt = take(KC * self.TOK * 4)
        self.o_cp = take(CP_COLS * 4)
        self.o_idb = take(512)
        self.o_ghn = take(D * 4)
        self.o_wg = take(512)
        self.o_wb = [take(TILE_EL * 2) for _ in range(NB)]
        self.o_persist = take(1024)
        self.o_arena = off
        self.total = off + self.ARENA
        TOK = self.TOK
        self.xT = self.mk(self.o_xt, F32, [KC, TOK])
        self.cp = self.mk(self.o_cp, F32, [CP_COLS])
        self.identb = self.mk(self.o_idb, BF16, [128])
        self.onesb = self.mk(self.o_idb + 256, BF16, [128])
        self.ghn = self.mk(self.o_ghn, F32, [D])
        self.wgs = [self.mk(self.o_wg, BF16, [KC, 8]), self.mk(self.o_wg + 256, BF16, [KC, 8])]
        self.wb_proj = [self.mk(o, BF16, [KC, 256]) for o in self.o_wb]
        self.wb_down = [self.mk(o, BF16, [16, 128]) for o in self.o_wb]
        pp = self.o_persist
        self.mpv = self.mk(pp, F32, [40])
        self.uprev = self.mk(pp + 256, F32, [KC, 2])
        self.min_ = self.mk(pp + 512, F32, [4])
        self.ident = self.cp[CP_IDENT:CP_IDENT + 128]
        self.maskneg = self.cp[CP_MASK:CP_MASK + 128]
        self.ones = self.cp[CP_ONES:CP_ONES + 128]
        self.neghalf = self.cp[CP_NEGHALF:CP_NEGHALF + 1]
        self.diag4 = self.cp[CP_DIAG4:CP_DIAG4 + 4].p(0, 4)
        self.negdiag4 = self.cp[CP_NEGDIAG4:CP_NEGDIAG4 + 4].p(0, 4)
        self.flag = self.cp[CP_FLAG:CP_FLAG + 1]
        a = self.o_arena
        self.RSTD = self.mk(a, F32, [BLK])
        self.TB = self.mk(a + 2048, F32, [BLK])
        self.SQ = [self.mk(a + 4096, F32, [BLK]), self.mk(a + 6144, F32, [BLK])]
        self.SQB = [self.mk(a + 4096, BF16, [BLK]), self.mk(a + 6144, BF16, [BLK])]
        self.HT = self.mk(a + 8192, BF16, [KC, BLK])
        b = a + 16384
        self.ACTT = self.mk(b, BF16, [16, BLK])
        self.YT_mlp = self.mk(b + 16384, F32, [KC, BLK])
        self.RT = [self.mk(a + 4096, F32, [BLK]), self.mk(a + 6144, F32, [BLK])]
        self.UT = self.mk(b, F32, [KC, BLK + 2])
        self.YT_conv = self.mk(b, F32, [KC, BLK])
        c0 = b + 16640
        self.CSB = [self.mk(c0, F32, [BLK]), self.mk(c0 + 2048, F32, [BLK])]
        self.ACC = [self.mk(c0 + 4096, F32, [BLK]), self.mk(c0 + 6144, F32, [BLK])]
        self.GT = self.mk(c0 + 8192, BF16, [KC, BLK])
        self.ULAST = self.mk(c0 + 16384, F32, [KC, 2])
        self.CSM = self.mk(c0 + 16384 + 256, F32, [16])
        self.HT2 = self.mk(c0 + 16384 + 512, BF16, [KC, 2])
        self.QT = self.mk(b, BF16, [NH, BLK])
        self.KT = self.mk(b + 4096, BF16, [NH, BLK])
        self.VAUG = self.mk(b + 8192, BF16, [4, NH, VA])
        self.VAUG2 = self.mk(b + 8192, BF16, [16, VA])
        self.YT_m = self.mk(b, F32, [KC, BLK])
        m0 = b + 16640
        self.GO = self.mk(m0, BF16, [4, D])
        m1 = m0 + 8192
        self.PT = self.mk(m1, BF16, [NH, CH])
        self.QS = self.mk(m1 + 1024, BF16, [NH, CH])
        self.KW = self.mk(m1 + 2048, BF16, [NH, CH])
        self.HG = self.mk(m1 + 3072, BF16, [D])
        self.OTMP = self.mk(m1 + 3072, F32, [256])
        self.JUNK = self.mk(m1 + 5120, BF16, [256])
        self.TOKS = self.mk(m1 + 5632, F32, [64])
        self.SM = self.mk(m1 + 5888, F32, [64])
        self.R3 = self.mk(m1 + 6144, F32, [64])
        p0 = b + 32768
        self.CST = self.mk(p0, F32, [NH, VA])
        self.CB = self.mk(p0 + 4352, BF16, [NH, VA])
        self.CIN = self.mk(m1, F32, [NH, VA])
        end = p0 + 4352 + 2304
        assert m1 + 6400 <= p0
        assert end - self.o_arena <= self.ARENA, (end - self.o_arena, self.ARENA)
        self.GA = self.RSTD
        self.GB = self.TB
        self.GC = self.SQ[0]
        self.GD = self.SQ[1]
        self.NBD = self.SQ[0]
        self.EB = self.SQ[1]
        self.IB = self.RSTD
        self.banks = [View(PS[:, i * 512:(i + 1) * 512], "ps", i * 2048, [512], [1], 4) for i in range(8)]
        self.banks_b = [View(PS[:, i * 512:(i + 1) * 512].bitcast(BF16), "ps", i * 2048, [1024], [1], 2)
                        for i in range(8)]

    ARENA = 16384 + 32768 + 4352 + 2304

    def mk(self, off, dt, fshape):
        esz = 4 if dt in (F32, F32R) else 2
        n = int(np.prod(fshape))
        nb = n * esz
        n4 = (nb + 3) // 4
        assert off % 4 == 0
        ap = self.S[:, off // 4: off // 4 + n4]
        if esz == 2:
            ap = ap.bitcast(BF16)[:, :n]
        if len(fshape) == 2:
            ap = ap.rearrange("p (a b) -> p a b", a=fshape[0])
        elif len(fshape) == 3:
            ap = ap.rearrange("p (a b c) -> p a b c", a=fshape[0], b=fshape[1])
        strides = []
        s = 1
        for d_ in reversed(fshape):
            strides.append(s)
            s *= d_
        return View(ap, "sb", off, fshape, list(reversed(strides)), esz)

    def bank(self):
        while True:
            self.bank_rr = (self.bank_rr + 1) % 8
            if self.bank_rr != getattr(self, "stats_bank", None):
                return self.bank_rr

    def stats_push(self, j, src):
        if j == 0:
            self.stats_bank = None
            self.stats_bank = self.bank()
            self.stats_pend = None
        self.stats_flush()
        sq = self.SQB[j % 2]
        self.act(sq, src, AF.Square)
        self.stats_pend = (j, sq)

    def stats_flush(self):
        if self.stats_pend is not None:
            j, sq = self.stats_pend
            self.mm(self.banks[self.stats_bank], self.onesb, sq, start=(j == 0), stop=(j == KC - 1))
            self.stats_pend = None

    def mm(self, out, lhsT, rhs, start=True, stop=True, sig=None):
        return self.P.add("pe", lambda e: e.matmul(out.ap, lhsT.ap, rhs.ap, start=start, stop=stop,
                                                   skip_group_check=True),
                          reads=[lhsT, rhs], writes=[out], force_signal=sig)

    def tr(self, out, in_, ident):
        return self.P.add("pe", lambda e: e.transpose(out.ap, in_.ap, ident.ap),
                          reads=[in_, ident], writes=[out])

    def act(self, out, in_, func, bias=None, scale=None, accum=None):
        reads = [in_]
        kw = {}
        if bias is not None:
            if isinstance(bias, View):
                reads.append(bias)
                kw["bias"] = bias.ap
            else:
                kw["bias"] = float(bias)
        if scale is not None:
            if isinstance(scale, View):
                reads.append(scale)
                kw["scale"] = scale.ap
            else:
                kw["scale"] = float(scale)
        writes = [out]
        if accum is not None:
            writes.append(accum)
            kw["accum_out"] = accum.ap
        return self.P.add("act", lambda e: e.activation(out.ap, in_.ap, func, **kw), reads=reads, writes=writes)

    def _eng(self, name):
        return name

    def tt(self, eng, out, in0, in1, op):
        return self.P.add(eng, lambda e: e.tensor_tensor(out.ap, in0.ap, in1.ap, op), reads=[in0, in1], writes=[out])

    def ts(self, eng, out, in0, s1, s2, op0, op1=None):
        reads = [in0]
        a1 = s1.ap if isinstance(s1, View) else s1
        a2 = s2.ap if isinstance(s2, View) else s2
        if isinstance(s1, View):
            reads.append(s1)
        if isinstance(s2, View):
            reads.append(s2)
        if op1 is None:
            return self.P.add(eng, lambda e: e.tensor_scalar(out.ap, in0.ap, a1, None, op0), reads=reads, writes=[out])
        return self.P.add(eng, lambda e: e.tensor_scalar(out.ap, in0.ap, a1, a2, op0, op1), reads=reads, writes=[out])

    def stt(self, out, in0, scalar, in1, op0, op1):
        reads = [in0, in1]
        a = scalar.ap if isinstance(scalar, View) else scalar
        if isinstance(scalar, View):
            reads.append(scalar)
        return self.P.add("dve", lambda e: e.scalar_tensor_tensor(out.ap, in0.ap, a, in1.ap, op0, op1),
                          reads=reads, writes=[out])

    def cp_(self, eng, out, in_):
        if eng == "act":
            return self.act(out, in_, AF.Copy)
        return self.P.add(eng, lambda e: e.tensor_copy(out.ap, in_.ap), reads=[in_], writes=[out])

    def recip(self, out, in_):
        return self.P.add("dve", lambda e: e.reciprocal(out.ap, in_.ap), reads=[in_], writes=[out])

    def scan(self, out, d0, d1, initial, op0, op1):
        reads = [d0, d1]
        a = initial.ap if isinstance(initial, View) else initial
        if isinstance(initial, View):
            reads.append(initial)
        return self.P.add("dve", lambda e: e.tensor_tensor_scan(out.ap, d0.ap, d1.ap, a, op0, op1),
                          reads=reads, writes=[out])

    def memset(self, eng, out, val):
        return self.P.add(eng, lambda e: e.memset(out.ap, val), writes=[out])

    def dma(self, q, out_ap, in_ap, sem, reads=(), writes=(), deps=(), slow=False):
        if slow:
            fn = lambda e: e.dma_start(out=out_ap, in_=in_ap, allow_slow_non_contiguous=True)
        else:
            fn = lambda e: e.dma_start(out=out_ap, in_=in_ap)
        return self.P.add(q, fn, reads=reads, writes=writes, sig=("dma", sem, 16), deps=deps)

    def step(self, tile, fn, tag=None):
        self.steps.append((tile, fn, getattr(self, "tagpfx", "") + (tag or getattr(fn, "__name__", "?"))))

    def run_steps(self):
        tiles = [(i, t) for i, (t, _, _) in enumerate(self.steps) if t is not None]
        issued = 0
        self.wviews = {}
        ti = 0
        for i, (t, fn, tag) in enumerate(self.steps):
            self.P.cur_tag = tag
            if t is not None:
                while issued < len(tiles) and issued < ti + NB:
                    si, tid = tiles[issued]
                    slot = issued % NB
                    deps = [self.cv_dep[tid]]
                    self.dma("sp", self.S[:, self.o_wb[slot] // 4: self.o_wb[slot] // 4 + TILE_EL // 2].bitcast(BF16),
                             self.wsc[tid], "wb%d" % slot, writes=[self.wb_proj[slot]], deps=deps)
                    self.wviews[si] = slot
                    issued += 1
                slot = self.wviews[i]
                ti += 1
                fn(self.wb_proj[slot], self.wb_down[slot])
            else:
                fn()

    def rms_stats(self, srcs, N, rstd, tb):
        bk = self.banks[self.bank()]
        for kc in range(KC):
            sq = self.SQB[kc % 2][0:N]
            self.act(sq, srcs[kc], AF.Square)
            self.mm(bk[0:N], self.onesb, sq, start=(kc == 0), stop=(kc == KC - 1))
        self.act(rstd[0:N], bk[0:N], AF.Ln, bias=self.eps_col, scale=1.0 / D)
        self.act(rstd[0:N], rstd[0:N], AF.Exp, scale=-0.5)

    def gcol(self, l, j, kc):
        c = CP_G + (l * 4 + j) * 8 + kc
        return self.cp[c:c + 1]

    def pre_norm(self, l, j, t0):
        srcs = [self.xT[kc, t0:t0 + BLK] for kc in range(KC)]
        self.rms_stats(srcs, BLK, self.RSTD, self.TB)
        for kc in range(KC):
            self.stt(self.HT[kc], srcs[kc], self.gcol(l, j, kc), self.RSTD, ALU.mult, ALU.mult)

    def post_norm_residual(self, l, j, t0, YT, fuse_pre=None, unnorm_in=False):
        self.stats_flush()
        bk = self.banks[self.stats_bank]
        self.stats_bank = None
        if unnorm_in:
            self.stt(self.RSTD, self.TB, EPS * D, bk, ALU.mult, ALU.add)
            self.act(self.RSTD, self.RSTD, AF.Ln, scale=1.0 / D)
        else:
            self.act(self.RSTD, bk, AF.Ln, bias=self.eps_col, scale=1.0 / D)
        self.act(self.RSTD, self.RSTD, AF.Exp, scale=-0.5)
        if fuse_pre is not None:
            bk2 = self.banks[self.bank()]
        for kc in range(KC):
            self.stt(YT[kc], YT[kc], self.gcol(l, j, kc), self.RSTD, ALU.mult, ALU.mult)
            xs = self.xT[kc, t0:t0 + BLK]
            self.tt("dve", xs, xs, YT[kc], ALU.add)
            if fuse_pre is not None:
                self.act(self.HT[kc], xs, AF.Copy, scale=self.gcol(l, fuse_pre, kc))
                sq = self.SQB[kc % 2]
                self.act(sq, xs, AF.Square)
                self.mm(bk2, self.onesb, sq, start=(kc == 0), stop=(kc == KC - 1))
        if fuse_pre is not None:
            self.act(self.TB, bk2, AF.Identity, bias=self.eps_col, scale=1.0 / D)
            self.tt("dve", self.TB, self.TB, self.TB, ALU.mult)

    def mlp_block(self, l, blk, prefetch=None, lite=None):
        t0 = blk * BLK
        base = l * TILES_PER_LAYER
        for hh in range(2):
            for w in range(8):
                def up(wp, wd, w=w):
                    for cc in range(2):
                        fc = w * 2 + cc
                        bk = self.banks[self.bank()]
                        for kc in range(KC):
                            self.mm(bk, wp[kc, cc * 128:(cc + 1) * 128], self.HT[kc], start=(kc == 0), stop=(kc == KC - 1))
                        rt = self.RT[fc % 2]
                        self.act(rt, bk, AF.Relu)
                        self.tt("dve", self.ACTT[fc], rt, rt, ALU.mult)
                self.step(base + 16 + hh * 8 + w, up)
            if hh == 1 and prefetch is not None:
                self.step(None, prefetch, tag="mix_prenorm")
            for j in range(8):
                def down(wp, wd, j=j, hh=hh):
                    bk = self.banks[self.bank()]
                    for fc in range(16):
                        self.mm(bk, wd[fc], self.ACTT[fc], start=(fc == 0), stop=(fc == 15))
                    if hh == 0:
                        self.act(self.YT_mlp[j], bk, AF.Copy)
                    else:
                        self.tt("dve", self.YT_mlp[j], bk, self.YT_mlp[j], ALU.add)
                        self.stats_push(j, self.YT_mlp[j])
                self.step(base + 32 + hh * 8 + j, down)
        if lite is not None:
            def lite_step():
                self.stats_flush()
                lite()
            self.step(None, lite_step, tag="mix_prenorm")
        self.step(None, lambda: self.post_norm_residual(l, 3, t0, self.YT_mlp, unnorm_in=True), tag="mlp_postnorm")

    def wc(self, lc, j, kc):
        c = CP_WC + (lc * 3 + j) * 8 + kc
        return self.cp[c:c + 1]

    def conv_layer(self, l):
        self.tagpfx = "C."
        lc = l // 2
        base = l * TILES_PER_LAYER
        TOK = self.TOK
        def halo_norm():
            srcs = [self.xT[kc, TOK - 2:TOK] for kc in range(KC)]
            self.rms_stats(srcs, 2, self.RSTD, self.TB)
            for kc in range(KC):
                self.stt(self.HT2[kc], srcs[kc], self.gcol(l, 0, kc), self.RSTD[0:2], ALU.mult, ALU.mult)
        self.step(None, halo_norm)
        hb = {}
        for w in range(8):
            def halo_proj(wp, wd, w=w):
                if w == 0:
                    hb["bk"] = self.banks[self.bank()]
                bk = hb["bk"]
                for cc in range(2):
                    col = (w * 2 + cc) * 2
                    for kc in range(KC):
                        self.mm(bk[col:col + 2], wp[kc, cc * 128:(cc + 1) * 128], self.HT2[kc],
                                start=(kc == 0), stop=(kc == KC - 1))
            self.step(base + 4 + w, halo_proj)

        def halo_finish():
            bk = hb["bk"]
            self.act(self.CSM, bk[0:16], AF.Copy)
            ul = self.ULAST
            self.tt("dve", ul.w(ul.ap.rearrange("p a b -> p (a b)")), self.CSM, bk[16:32], ALU.mult)
            nc = self.nc
            bnc, gth = self.cbounce[lc], self.cgath[lc]
            d1 = self.dma("sp", bnc[:, :], ul.ap.rearrange("p a b -> p (a b)"), "xs", reads=[ul])
            hb["cc"] = self.P.add("pool", lambda e: e.collective_compute(
                "AllGather", ALU.bypass, replica_groups=[[0, 1], [2, 3], [4, 5], [6, 7]],
                ins=[bnc], outs=[gth]), sig=("dma", "cc", 1), deps=[d1])
            self.convert_next(l)
        self.step(None, halo_finish)

        def halo_recv():
            gth = self.cgath[lc]
            up = self.uprev
            self.dma("sp", up.ap.rearrange("p a b -> p (a b)"), gth[0:128, :], "xr", writes=[up], deps=[hb["cc"]])
            self.ts("dve", up, up, self.flag, None, ALU.mult)

        for blk in range(self.NBLK):
            t0 = blk * BLK
            if blk == 0 or not (PREFETCH or PREFETCH_LITE):
                self.step(None, lambda t0=t0: self.pre_norm(l, 0, t0), tag="mix_prenorm")
            st = {}
            for w in range(4):
                def cproj(wp, wd, w=w):
                    for cc in range(2):
                        bk = self.banks[self.bank()]
                        for kc in range(KC):
                            self.mm(bk, wp[kc, cc * 128:(cc + 1) * 128], self.HT[kc], start=(kc == 0), stop=(kc == KC - 1))
                        self.act(self.CSB[cc], bk, AF.Copy)
                self.step(base + 4 + w, cproj)

                def xproj(wp, wd, w=w):
                    for cc in range(2):
                        j = w * 2 + cc
                        bk = self.banks[self.bank()]
                        for kc in range(KC):
                            self.mm(bk, wp[kc, cc * 128:(cc + 1) * 128], self.HT[kc], start=(kc == 0), stop=(kc == KC - 1))
                        self.tt("dve", self.UT[j, 2:BLK + 2], bk, self.CSB[cc], ALU.mult)
                self.step(base + 8 + w, xproj)
            if blk == 0:
                self.step(None, halo_recv)
            for w in range(4):
                def bproj(wp, wd, w=w):
                    if w == 0:
                        self.cp_("dve", self.UT[:, 0:2], self.uprev)
                    for cc in range(2):
                        j = w * 2 + cc
                        bk = self.banks[self.bank()]
                        for kc in range(KC):
                            self.mm(bk, wp[kc, cc * 128:(cc + 1) * 128], self.HT[kc], start=(kc == 0), stop=(kc == KC - 1))
                        acc = self.ACC[cc]
                        self.ts("dve", acc, self.UT[j, 2:BLK + 2], self.wc(lc, 2, j), None, ALU.mult)
                        self.stt(acc, self.UT[j, 1:BLK + 1], self.wc(lc, 1, j), acc, ALU.mult, ALU.add)
                        self.stt(acc, self.UT[j, 0:BLK], self.wc(lc, 0, j), acc, ALU.mult, ALU.add)
                        self.tt("dve", self.GT[j], bk, acc, ALU.mult)
                    if w == 3:
                        self.cp_("dve", self.uprev, self.UT[:, BLK:BLK + 2])
                self.step(base + w, bproj)
            for w in range(4):
                def oproj(wp, wd, w=w):
                    for cc in range(2):
                        j = w * 2 + cc
                        bk = self.banks[self.bank()]
                        for kc in range(KC):
                            self.mm(bk, wp[kc, cc * 128:(cc + 1) * 128], self.GT[kc], start=(kc == 0), stop=(kc == KC - 1))
                        self.act(self.YT_conv[j], bk, AF.Copy)
                        self.stats_push(j, self.YT_conv[j])
                self.step(base + 12 + w, oproj)
            self.step(None, lambda t0=t0: self.post_norm_residual(l, 1, t0, self.YT_conv, fuse_pre=2), tag="mix_postnorm")
            nxt = (lambda t1=t0 + BLK: self.pre_norm(l, 0, t1)) if (PREFETCH and blk + 1 < self.NBLK) else None
            lite = (lambda t1=t0 + BLK: self.pre_norm(l, 0, t1)) if (PREFETCH_LITE and blk + 1 < self.NBLK) else None
            self.mlp_block(l, blk, prefetch=nxt, lite=lite)

    def bgate(self, lm, which):
        c = CP_BG + lm * 2 + which
        return self.cp[c:c + 1].p(0, 4)

    def gate_rows(self, lm, blk, gi_bank, gf_bank, main):
        self.gate_rows_a(lm, blk, gi_bank, gf_bank, main)
        self.gate_rows_b(lm, blk, main)

    def gate_rows_a(self, lm, blk, gi_bank, gf_bank, main):
        A, B, C_, Dd = (self.GA.p(0, 4), self.GB.p(0, 4), self.GC.p(0, 4), self.GD.p(0, 4))
        self.ts("dve", A, gi_bank.p(0, 4), self.bgate(lm, 0), None, ALU.add)
        self.ts("dve", B, gf_bank.p(0, 4), self.bgate(lm, 1), None, ALU.add)
        self.act(A, A, AF.Tanh, scale=1.0 / CAP)
        self.act(B, B, AF.Tanh, scale=1.0 / CAP)
        self.act(B, B, AF.Exp, scale=-CAP)
        self.act(B, B, AF.Ln, bias=self.one_col.p(0, 4))
        ones_row = self.ones.p(0, 4)
        for c in range(4):
            cs = slice(c * CH, (c + 1) * CH)
            self.scan(C_[cs], ones_row, B[cs], 0.0, ALU.mult, ALU.subtract)
        self.stt(A, A, CAP, C_, ALU.mult, ALU.subtract)
        for c in range(4):
            cs = slice(c * CH, (c + 1) * CH)
            gc = blk * 4 + c
            self.scan(B[cs], A[cs], A[cs], self.mpv[gc:gc + 1].p(0, 4), ALU.max, ALU.max)
            self.tt("dve", self.mpv[gc + 1:gc + 2].p(0, 4), C_[c * CH + CH - 1:c * CH + CH], B[c * CH + CH - 1:c * CH + CH], ALU.add)
        A3 = A.ap.rearrange("p (c t) -> p c t", c=4)
        B3 = B.ap.rearrange("p (c t) -> p c t", c=4)
        D3 = Dd.ap.rearrange("p (c t) -> p c t", c=4)
        self.tt("dve", Dd.w(D3), A.w(A3), B.w(B3[:, :, CH - 1:CH].to_broadcast([4, 4, CH])), ALU.subtract)
        self.act(Dd, Dd, AF.Exp)
        if main:
            self.tt("dve", C_, C_, B, ALU.add)
            self.act(C_, C_, AF.Exp, scale=-1.0)

    def gate_rows_b(self, lm, blk, main):
        A, B, C_, Dd = (self.GA.p(0, 4), self.GB.p(0, 4), self.GC.p(0, 4), self.GD.p(0, 4))
        B3 = B.ap.rearrange("p (c t) -> p c t", c=4)
        mb = self.banks[7]
        qs = [(A, 0), (Dd, 1)] + ([(C_, 2)] if main else [])
        for c in range(4):
            for row, qi in qs:
                col = (c * 3 + qi) * 4
                self.mm(mb[col:col + 4], row[c * CH:(c + 1) * CH], self.diag4, start=True, stop=True)
        r3 = self.R3.p(0, 4)
        g0 = blk * 4
        self.tt("dve", r3[0:4], self.mpv[g0:g0 + 4].p(0, 4), B.w(B3[:, :, CH - 1]), ALU.subtract)
        r16 = r3[4:20]
        self.tt("dve", r16.w(r16.ap.rearrange("p (c h) -> p c h", c=4)),
                r3[0:4].w(r3[0:4].ap.unsqueeze(2).to_broadcast([4, 4, 4])),
                self.diag4.w(self.diag4.ap.unsqueeze(1).to_broadcast([4, 4, 4])), ALU.mult)
        self.mm(mb[48:64], self.ones.p(0, 4), r16, start=True, stop=True)
        self.cp_("dve", self.TOKS[0:48], mb[0:48])
        self.act(self.TOKS[48:64], mb[48:64], AF.Exp)

    def tok(self, c, qi, h):
        col = (c * 3 + qi) * 4 + h
        return self.TOKS[col:col + 1]

    def state_update(self, c, main):
        self.state_kw(c)
        self.state_kv(c, main)

    def state_kw(self, c):
        kb = self.banks_b[6]
        for h in range(NH):
            self.tr(kb[h * CH:(h + 1) * CH], self.KT[h, c * CH:(c + 1) * CH], self.identb)
        for h in range(NH):
            if h % 2 == 0:
                self.act(self.KW[h], kb[h * CH:(h + 1) * CH], AF.Copy, scale=self.tok(c, 1, h))
            else:
                self.ts("dve", self.KW[h], kb[h * CH:(h + 1) * CH], self.tok(c, 1, h), None, ALU.mult)

    def state_kv(self, c, main):
        for h in range(NH):
            nb = self.banks[h]
            self.mm(nb[0:VA], self.KW[h], self.VAUG[c, h], start=True, stop=True)
        for h in range(NH):
            nb = self.banks[h]
            il = self.TOKS[48 + c * 4 + h:48 + c * 4 + h + 1]
            self.stt(self.CST[h], self.CST[h], il, nb[0:VA], ALU.mult, ALU.add)
        if main:
            self.cp_("act", self.CB, self.CST)

    def proj_feat(self, wp, dst_fn, evac):
        for cc in range(2):
            bk = self.banks[self.bank()]
            for kc in range(KC):
                self.mm(bk, wp[kc, cc * 128:(cc + 1) * 128], self.HT[kc], start=(kc == 0), stop=(kc == KC - 1))
            evac(cc, bk)

    def proj_tok(self, wp, evac):
        for c in range(4):
            bi = self.bank()
            bk = self.banks[bi]
            half = bk[0:256]
            for kc in range(KC):
                self.mm(half, self.HT[kc, c * CH:(c + 1) * CH], wp[kc], start=(kc == 0), stop=(kc == KC - 1))
            evac(c, half)

    def gates_proj(self, st, lm):
        self.wg = self.wgs[lm]
        gi = self.banks[self.bank()]
        gf = self.banks[self.bank()]
        for kc in range(KC):
            self.mm(gi.p(0, 4), self.wg[kc, 0:4], self.HT[kc], start=(kc == 0), stop=(kc == KC - 1))
        for kc in range(KC):
            self.mm(gf.p(0, 4), self.wg[kc, 4:8], self.HT[kc], start=(kc == 0), stop=(kc == KC - 1))
        st["gi"], st["gf"] = gi, gf

    def mlstm_layer(self, l):
        lm = l // 2
        base = l * TILES_PER_LAYER
        nc = self.nc
        w_in = self.w_in_m

        def layer_init():
            self.dma("sp", self.ghn.ap, self.ghn_d[lm], "ghn", writes=[self.ghn])
            self.ts("dve", self.ghn, self.ghn, 0.5, None, ALU.mult)
            self.memset("dve", self.CST, 0.0)
            self.memset("dve", self.mpv.p(0, 4), 0.0)
        self.step(None, layer_init)

        self.tagpfx = "P."
        for blk in range(self.NBLK):
            t0 = blk * BLK
            st = {}
            if blk == 0 or not PREFETCH:
                self.step(None, lambda t0=t0: self.pre_norm(l, 0, t0), tag="mix_prenorm")
            for w in range(2):
                def kproj(wp, wd, w=w):
                    def ev(cc, bk):
                        self.act(self.KT[w * 2 + cc], bk, AF.Copy, scale=128.0 ** -0.5)
                    self.proj_feat(wp, None, ev)
                self.step(base + 2 + w, kproj)
            for w in range(4):
                def vproj(wp, wd, w=w, st=st, blk=blk):
                    if w == 0:
                        self.memset("dve", self.VAUG2[:, 256:257], 1.0)
                        self.gates_proj(st, lm)
                        self.gate_rows_a(lm, blk, st["gi"], st["gf"], main=False)
                    def ev(c, half):
                        self.act(self.VAUG[c, w, 0:256], half, AF.Copy)
                    self.proj_tok(wp, ev)
                self.step(base + 4 + w, vproj)

            def pre_rows(blk=blk):
                self.gate_rows_b(lm, blk, main=False)
            self.step(None, pre_rows, tag="pre_chunks")
            if PREFETCH and blk + 1 < self.NBLK:
                self.step(None, lambda t1=t0 + BLK: self.pre_norm(l, 0, t1), tag="mix_prenorm")

            def pre_chunks():
                for c in range(4):
                    self.state_update(c, main=False)
            self.step(None, pre_chunks)

        xs = {}

        def exchange_send():
            bnc, gth = self.mbounce[lm], self.mgath[lm]
            cflat = self.CST.ap.rearrange("p h v -> p (h v)")
            d1 = self.dma("sp", bnc[:, 0:NH * VA], cflat, "xs", reads=[self.CST])
            gl = self.NBLK * 4
            d2 = self.dma("sp", bnc[0:4, NH * VA:NH * VA + 1], self.mpv[gl:gl + 1].p(0, 4).ap, "xs",
                          reads=[self.mpv[gl:gl + 1]], slow=True)
            xs["cc"] = self.P.add("pool", lambda e: e.collective_compute(
                "AllGather", ALU.bypass, replica_groups=[[0, 1], [2, 3], [4, 5], [6, 7]],
                ins=[bnc], outs=[gth]), sig=("dma", "cc", 1), deps=[d1, d2])
            self.convert_next(l)
        self.step(None, exchange_send)
        self.tagpfx = "M."

        def exchange_recv():
            gth = self.mgath[lm]
            cc = xs["cc"]
            self.dma("sp", self.CIN.ap.rearrange("p h v -> p (h v)"), gth[0:128, 0:NH * VA], "xr",
                     writes=[self.CIN], deps=[cc])
            self.dma("sp", self.min_[0:1].p(0, 4).ap, gth[0:4, NH * VA:NH * VA + 1], "xr2",
                     writes=[self.min_[0:1]], deps=[cc], slow=True)
            self.ts("dve", self.CST, self.CIN, self.flag, None, ALU.mult)
            self.cp_("act", self.CB, self.CST)
            self.ts("dve", self.mpv[0:1].p(0, 4), self.min_[0:1].p(0, 4), self.flag.p(0, 4), None, ALU.mult)

        for blk in range(self.NBLK):
            t0 = blk * BLK
            st = {}
            if blk == 0 or not (PREFETCH or PREFETCH_LITE):
                self.step(None, lambda t0=t0: self.pre_norm(l, 0, t0), tag="mix_prenorm")
            for w in range(2):
                def qproj(wp, wd, w=w):
                    def ev(cc, bk):
                        self.act(self.QT[w * 2 + cc], bk, AF.Copy)
                    self.proj_feat(wp, None, ev)
                self.step(base + w, qproj)
            for w in range(2):
                def kproj(wp, wd, w=w):
                    def ev(cc, bk):
                        self.act(self.KT[w * 2 + cc], bk, AF.Copy, scale=128.0 ** -0.5)
                    self.proj_feat(wp, None, ev)
                self.step(base + 2 + w, kproj)
            for w in range(4):
                def vproj(wp, wd, w=w, st=st, blk=blk):
                    if w == 0:
                        self.memset("dve", self.VAUG2[:, 256:257], 1.0)
                    def ev(c, half):
                        self.act(self.VAUG[c, w, 0:256], half, AF.Copy)
                    self.proj_tok(wp, ev)
                self.step(base + 4 + w, vproj)
            for w in range(4):
                def oproj(wp, wd, w=w, st=st, blk=blk):
                    if w == 0 and blk > 0:
                        self.gates_proj(st, lm)
                        self.gate_rows_a(lm, blk, st["gi"], st["gf"], main=True)
                    if w == 2 and blk > 0:
                        self.gate_rows_b(lm, blk, main=True)
                        self.chunk_s1(blk, 0)

                    def ev(c, half):
                        tmp = self.OTMP
                        self.act(tmp, half, AF.Tanh, scale=0.5)
                        self.stt(self.GO[c, w * 256:(w + 1) * 256], tmp, 1.0, self.ghn[w * 256:(w + 1) * 256],
                                 ALU.add, ALU.mult)
                    self.proj_tok(wp, ev)
                self.step(base + 8 + w, oproj)

            if blk == 0:
                self.step(None, exchange_recv)

            def chunks(blk=blk, st=st):
                if blk == 0:
                    self.gates_proj(st, lm)
                    self.gate_rows_a(lm, blk, st["gi"], st["gf"], main=True)
                    self.gate_rows_b(lm, blk, main=True)
                    self.chunk_s1(blk, 0)
                for c in range(4):
                    self.chunk_s2a(blk, c)
                    if c < 3:
                        self.chunk_s1(blk, c + 1)
                    self.state_kw(c)
                    self.chunk_s2b(blk, c)
                    if c < 3:
                        self.state_kv(c, main=True)
                        self.chunk_s2c(blk, c)
                    else:
                        self.chunk_s2c(blk, c)
                        self.state_kv(c, main=True)
            self.step(None, chunks)
            for w in range(4):
                def outproj(wp, wd, w=w):
                    for cc in range(2):
                        j = w * 2 + cc
                        bk = self.banks[self.bank()]
                        for kc in range(KC):
                            self.mm(bk, wp[kc, cc * 128:(cc + 1) * 128], self.HT[kc], start=(kc == 0), stop=(kc == KC - 1))
                        self.act(self.YT_m[j], bk, AF.Copy)
                        self.stats_push(j, self.YT_m[j])
                self.step(base + 12 + w, outproj)
            self.step(None, lambda t0=t0: self.post_norm_residual(l, 1, t0, self.YT_m, fuse_pre=2), tag="mix_postnorm")
            nxt = (lambda t1=t0 + BLK: self.pre_norm(l, 0, t1)) if (PREFETCH and blk + 1 < self.NBLK) else None
            lite = (lambda t1=t0 + BLK: self.pre_norm(l, 0, t1)) if (PREFETCH_LITE and blk + 1 < self.NBLK) else None
            self.mlp_block(l, blk, prefetch=nxt, lite=lite)

    def chunk_s1(self, blk, c):
        gc = blk * 4 + c
        cs = slice(c * CH, (c + 1) * CH)
        Mrow = self.GB.p(0, 4)
        nbd = self.NBD.p(0, 4)
        nbd3 = nbd.ap.rearrange("p (h t) -> p h t", h=4)
        self.tt("dve", nbd.w(nbd3), Mrow[cs].w(Mrow[cs].ap.unsqueeze(1).to_broadcast([4, 4, CH])),
                self.negdiag4.w(self.negdiag4.ap.unsqueeze(2).to_broadcast([4, 4, CH])), ALU.mult)
        BC, BC2, SS = self.banks[4], self.banks[5], self.banks[6]
        ones4 = self.ones.p(0, 4)
        self.mm(BC, ones4, nbd, start=True, stop=True)
        self.stt(nbd.w(nbd3), self.diag4.w(self.diag4.ap.unsqueeze(2).to_broadcast([4, 4, CH])),
                 self.mpv[gc:gc + 1].p(0, 4), nbd.w(nbd3), ALU.mult, ALU.add)
        self.mm(BC2, ones4, nbd, start=True, stop=True)
        for h in range(NH):
            self.mm(SS[h * CH:(h + 1) * CH], self.KT[h, cs], self.QT[h, cs], start=True, stop=True)
        E = self.EB
        for h in range(NH):
            self.stt(E[h * CH:(h + 1) * CH], BC[h * CH:(h + 1) * CH], self.tok(c, 0, h), self.maskneg, ALU.add, ALU.min)
        self.act(E, E, AF.Exp)
        self.tt("dve", self.PT.w(self.PT.ap.rearrange("p h t -> p (h t)")), SS, E, ALU.mult)
        self.act(self.IB, BC2, AF.Exp)
        ib3 = self.IB.ap.rearrange("p (h t) -> p h t", h=4)
        self.tt("dve", self.QS, self.QT[:, cs], self.IB.w(ib3), ALU.mult)

    def chunk_s2a(self, blk, c):
        for h in range(NH):
            self.mm(self.banks[h][0:VA], self.PT[h], self.VAUG[c, h], start=True, stop=False)
        for h in range(NH):
            self.mm(self.banks[h][0:VA], self.QS[h], self.CB[h], start=False, stop=True)

    def chunk_s2b(self, blk, c):
        cs = slice(c * CH, (c + 1) * CH)
        sm = self.SM
        dmax, r, ssq, t4, rs2, sc = (sm[0:4], sm[4:8], sm[8:12], sm[12:16], sm[16:20], sm[20:24])
        den_ap = self.PS[:, 0:4 * 512].rearrange("p (b c) -> p b c", b=4)[:, :, 256]
        den = View(den_ap, "ps", 0, [4 * 512], [1], 4)
        fl = self.TOKS[(c * 3 + 2) * 4:(c * 3 + 2) * 4 + 4]
        self.act(dmax, den, AF.Abs)
        for h in range(NH):
            self.act(self.JUNK, self.banks[h][0:256], AF.Square, accum=ssq[h:h + 1])
        self.tt("dve", dmax, dmax, fl, ALU.max)
        self.recip(r, dmax)
        self.tt("dve", t4, r, r, ALU.mult)
        self.tt("dve", t4, t4, ssq, ALU.mult)
        self.act(t4, t4, AF.Ln, bias=self.eps_col, scale=1.0 / 256)
        self.act(rs2, t4, AF.Exp, scale=-0.5)
        self.tt("dve", sc, r, rs2, ALU.mult)
        for h in range(NH):
            self.stt(self.HG[h * 256:(h + 1) * 256], self.banks[h][0:256], sc[h:h + 1],
                     self.GO[c, h * 256:(h + 1) * 256], ALU.mult, ALU.mult)

    def chunk_s2c(self, blk, c):
        cs = slice(c * CH, (c + 1) * CH)
        tb_ = self.banks_b[7]
        for j in range(KC):
            self.tr(tb_[j * CH:(j + 1) * CH], self.HG[j * CH:(j + 1) * CH], self.identb)
        hgt = self.HT[:, cs]
        self.act(hgt, tb_.w(tb_.ap.rearrange("p (j t) -> p j t", j=KC)), AF.Copy)

    def build(self):
        nc = self.nc
        TOK = self.TOK
        dt = nc.dram_tensor
        self.xT_d = dt("xT", [D, TOK], F32, kind="ExternalInput").ap()
        self.cp_d = dt("cpack", [128, CP_COLS], F32, kind="ExternalInput").ap()
        self.ghn_d = dt("ghn", [2, 128, D], F32, kind="ExternalInput").ap()
        self.w_in_m = dt("w_in_mlstm", [2, D, 3080], F32, kind="ExternalInput").ap()
        self.w_out_m = dt("w_out_mlstm", [2, D, D], F32, kind="ExternalInput").ap()
        self.w_in_c = dt("w_in_conv", [2, D, 3072], F32, kind="ExternalInput").ap()
        self.w_out_c = dt("w_out_conv", [2, D, D], F32, kind="ExternalInput").ap()
        self.w_up = dt("w_mlp_up", [4, D, DFF], F32, kind="ExternalInput").ap()
        self.w_dn = dt("w_mlp_down", [4, DFF, D], F32, kind="ExternalInput").ap()
        self.yT_d = dt("yT", [D, TOK], F32, kind="ExternalOutput").ap()
        self.wsc = dt("wsc", [4 * TILES_PER_LAYER, 128, TILE_EL], BF16, kind="Internal").ap()
        self.mbounce = [dt("mb%d" % i, [128, 1040], F32, kind="Internal").ap() for i in range(2)]
        self.mgath = [dt("mg%d" % i, [256, 1040], F32, kind="Internal").ap() for i in range(2)]
        self.cbounce = [dt("cb%d" % i, [128, 16], F32, kind="Internal").ap() for i in range(2)]
        self.cgath = [dt("cg%d" % i, [256, 16], F32, kind="Internal").ap() for i in range(2)]

        with contextlib.ExitStack() as es:
            tot_probe = KC * TOK * 4
            self.S = None
            dummy_total = self._layout_total()
            S = es.enter_context(nc.sbuf_tensor("S", [128, dummy_total // 4], F32))
            PS = es.enter_context(nc.psum_tensor("PS", [128, 4096], F32))
            self.setup_memory(S, PS)
            assert self.total == dummy_total
            self.eps_col = self.mk(self.o_persist + 768, F32, [1])
            self.one_col = self.mk(self.o_persist + 772, F32, [1])
            self.program()
            self.P.finalize()
            sems = {n: es.enter_context(nc.semaphore(n)) for n in self.P.sem_names()}
            block = es.enter_context(nc.Block())
            P = self.P

            @block.tensor
            def _(e):
                P.emit("pe", e, sems)

            @block.scalar
            def _(e):
                P.emit("act", e, sems)

            @block.vector
            def _(e):
                P.emit("dve", e, sems)

            @block.gpsimd
            def _(e):
                P.emit("pool", e, sems)

            @block.sync
            def _(e):
                P.emit("sp", e, sems)
        return nc

    def _layout_total(self):
        off = 0

        def take(nbytes):
            nonlocal off
            off += (nbytes + CELL - 1) // CELL * CELL
        take(KC * self.TOK * 4)
        take(CP_COLS * 4)
        take(512)
        take(D * 4)
        take(512)
        for _ in range(NB):
            take(TILE_EL * 2)
        take(1024)
        return off + self.ARENA

    def convert_layer(self, l):
        lidx = l // 2
        base = l * TILES_PER_LAYER
        if l % 2 == 0:
            win, wout = self.w_in_m[lidx], self.w_out_m[lidx]
        else:
            win, wout = self.w_in_c[lidx], self.w_out_c[lidx]
        winr = win.rearrange("(kc p) c -> p kc c", p=128)
        woutr = wout.rearrange("(kc p) c -> p kc c", p=128)
        wupr = self.w_up[l].rearrange("(kc p) c -> p kc c", p=128)
        wdnr = self.w_dn[l].rearrange("(fc p) c -> p fc c", p=128)
        first = (l == self.layers[0]) and l % 2 == 0
        order = ([2, 3, 4, 5, 6, 7] + [t for t in range(TILES_PER_LAYER) if t not in (2, 3, 4, 5, 6, 7)]) if first \
            else list(range(TILES_PER_LAYER))
        ga = []
        for n_, t in enumerate(order):
            if t < 12:
                src = winr[:, :, t * 256:(t + 1) * 256]
                dst = self.wsc[base + t].rearrange("p (a b) -> p a b", a=KC)
            elif t < 16:
                src = woutr[:, :, (t - 12) * 256:(t - 11) * 256]
                dst = self.wsc[base + t].rearrange("p (a b) -> p a b", a=KC)
            elif t < 32:
                src = wupr[:, :, (t - 16) * 256:(t - 15) * 256]
                dst = self.wsc[base + t].rearrange("p (a b) -> p a b", a=KC)
            else:
                hh, j = divmod(t - 32, 8)
                src = wdnr[:, hh * 16:(hh + 1) * 16, j * 128:(j + 1) * 128]
                dst = self.wsc[base + t].rearrange("p (a b) -> p a b", a=16)
            early = first and n_ < 6
            d_ = self.dma("pool", dst, src, ("cva%d" if early else "cv%d") % l)
            if early:
                ga.append(base + t)
                for tt_ in ga:
                    self.cv_dep[tt_] = d_
            else:
                for t2 in order[(6 if first else 0):]:
                    self.cv_dep[base + t2] = d_

    def convert_next(self, l):
        i = self.layers.index(l)
        if i + 1 < len(self.layers):
            self.convert_layer(self.layers[i + 1])

    def program(self):
        TOK = self.TOK
        self.dma("sp", self.cp.ap, self.cp_d, "cpl", writes=[self.cp])
        xr = self.xT_d.rearrange("(kc p) t -> p kc t", p=128)
        for kc in range(KC):
            self.dma("sp", self.xT[kc].ap, xr[:, kc, :], "xl%d" % kc, writes=[self.xT[kc]])
        self.cp_("dve", self.identb, self.ident)
        self.cp_("dve", self.onesb, self.ones)
        self.memset("dve", self.eps_col, EPS)
        self.memset("dve", self.one_col, 1.0)
        for lm in range(2):
            if 2 * lm in self.layers:
                self.dma("pool", self.wgs[lm].ap,
                         self.w_in_m[lm].rearrange("(kc p) c -> p kc c", p=128)[:, :, 3072:3080], "wgl%d" % lm,
                         writes=[self.wgs[lm]])
        self.cv_dep = {}
        self.convert_layer(self.layers[0])
        for l in self.layers:
            if l % 2 == 0:
                self.mlstm_layer(l)
            else:
                self.conv_layer(l)
        self.run_steps()
        yr = self.yT_d.rearrange("(kc p) t -> p kc t", p=128)
        outs = []
        for kc in range(KC):
            outs.append(self.dma("sp", yr[:, kc, :], self.xT[kc].ap, "xl%d" % kc, reads=[self.xT[kc]]))
        self.P.add("sp", None, sig=None, deps=outs)


def build_nc(TOK, layers):
    nc = bass.Bass("TRN2", target_bir_lowering=False)
    k = K(nc, TOK, layers)
    k.build()
    return nc, k


def make_cpack(norm_g, w_conv, b_gates, flag):
    cp = np.zeros((128, CP_COLS), np.float32)
    cp[:, CP_IDENT:CP_IDENT + 128] = np.eye(128, dtype=np.float32)
    s = np.arange(128)[:, None]
    t = np.arange(128)[None, :]
    cp[:, CP_MASK:CP_MASK + 128] = np.where(s <= t, 0.0, -30000.0).astype(np.float32)
    cp[:, CP_ONES:CP_ONES + 128] = 1.0
    cp[:, CP_G:CP_G + 128] = norm_g.reshape(4, 4, KC, 128).transpose(3, 0, 1, 2).reshape(128, 128)
    cp[:, CP_WC:CP_WC + 48] = w_conv.reshape(2, 3, KC, 128).transpose(3, 0, 1, 2).reshape(128, 48)
    cp[:, CP_NEGHALF] = -0.5
    cp[0:4, CP_DIAG4:CP_DIAG4 + 4] = np.eye(4, dtype=np.float32)
    cp[0:4, CP_NEGDIAG4:CP_NEGDIAG4 + 4] = -np.eye(4, dtype=np.float32)
    for lm in range(2):
        cp[0:4, CP_BG + lm * 2 + 0] = b_gates[lm, 0:4]
        cp[0:4, CP_BG + lm * 2 + 1] = b_gates[lm, 4:8]
    cp[:, CP_FLAG] = flag
    return cp


_CACHE = {}
LAST = {}


def run(x, norm_g, w_in_mlstm, b_gates_mlstm, g_hnorm, w_out_mlstm, w_in_conv, w_conv, w_out_conv,
        w_mlp_up, w_mlp_down, layers=(0, 1, 2, 3), trace=False):
    B, S, _ = x.shape
    assert B * 2 == NCORES
    TOK = S // 2
    key = (TOK, tuple(layers))
    if key not in _CACHE:
        _CACHE[key] = build_nc(TOK, list(layers))[0]
    nc = _CACHE[key]
    f = lambda a: np.ascontiguousarray(np.asarray(a, dtype=np.float32))
    ghn = np.ascontiguousarray(np.broadcast_to(f(g_hnorm)[:, None, :], (2, 128, D)))
    shared = {
        "ghn": ghn,
        "w_in_mlstm": f(w_in_mlstm), "w_out_mlstm": f(w_out_mlstm),
        "w_in_conv": f(w_in_conv), "w_out_conv": f(w_out_conv),
        "w_mlp_up": f(w_mlp_up), "w_mlp_down": f(w_mlp_down),
    }
    xf = f(x)
    in_maps = []
    for c in range(NCORES):
        b, half = divmod(c, 2)
        m = dict(shared)
        m["xT"] = np.ascontiguousarray(xf[b, half * TOK:(half + 1) * TOK, :].T)
        m["cpack"] = make_cpack(f(norm_g), f(w_conv), f(b_gates_mlstm), float(half))
        in_maps.append(m)
    res = run_bass_kernel_spmd(nc, in_maps, core_ids=list(range(NCORES)), **({"trace": True} if trace else {}))
    LAST["exec_ns"] = getattr(res, "exec_time_ns", None)
    out = np.empty((B, S, D), np.float32)
    for c in range(NCORES):
        b, half = divmod(c, 2)
        out[b, half * TOK:(half + 1) * TOK, :] = res.results[c]["yT"].T
    return out


def kernel(x, norm_g, w_in_mlstm, b_gates_mlstm, g_hnorm, w_out_mlstm, w_in_conv, w_conv, w_out_conv,
           w_mlp_up, w_mlp_down):
    return run(x, norm_g, w_in_mlstm, b_gates_mlstm, g_hnorm, w_out_mlstm, w_in_conv, w_conv, w_out_conv,
               w_mlp_up, w_mlp_down)
```

```python
import contextlib
import numpy as np
import concourse.bass as bass
import concourse.mybir as mybir
from concourse.bass_utils import run_bass_kernel_spmd

F32 = mybir.dt.float32
F32R = mybir.dt.float32r
BF16 = mybir.dt.bfloat16
AF = mybir.ActivationFunctionType
ALU = mybir.AluOpType

D = 1024
KC = 8
NH = 4
DFF = 4096
EPS = 1e-6
CAP = 15.0
NCORES = 8
BLK = 512
CH = 128
TILE_EL = 2048
NB = 4
PREFETCH_LITE = True
PREFETCH = False
TILES_PER_LAYER = 48
VA = 257
CELL = 256


CP_IDENT = 0
CP_MASK = 128
CP_ONES = 256
CP_G = 384
CP_WC = 512
CP_NEGHALF = 560
CP_DIAG4 = 562
CP_NEGDIAG4 = 566
CP_BG = 570
CP_FLAG = 574
CP_COLS = 576


class View:
    __slots__ = ("ap", "space", "base", "fshape", "fstr", "esz")

    def __init__(self, ap, space, base, fshape, fstr, esz):
        self.ap = ap
        self.space = space
        self.base = base
        self.fshape = list(fshape)
        self.fstr = list(fstr)
        self.esz = esz

    @property
    def lo(self):
        return self.base

    @property
    def hi(self):
        return self.base + (sum((n - 1) * s for n, s in zip(self.fshape, self.fstr)) + 1) * self.esz

    def __getitem__(self, idx):
        if not isinstance(idx, tuple):
            idx = (idx,)
        idx = idx + (slice(None),) * (len(self.fshape) - len(idx))
        base = self.base
        fshape, fstr = [], []
        for i, n, s in zip(idx, self.fshape, self.fstr):
            if isinstance(i, int):
                base += i * s * self.esz
            else:
                st, sp, step = i.indices(n)
                assert step == 1
                base += st * s * self.esz
                fshape.append(sp - st)
                fstr.append(s)
        return View(self.ap[(slice(None),) + idx], self.space, base, fshape, fstr, self.esz)

    def p(self, p0, p1):
        return View(self.ap[p0:p1], self.space, self.base, self.fshape, self.fstr, self.esz)

    def w(self, ap):
        return View(ap, self.space, self.base, self.fshape, self.fstr, self.esz)


class Ins:
    __slots__ = ("q", "fn", "raw", "oth", "sig", "epos", "signaled", "ev", "waits", "gidx", "tag")

    def __init__(self, q, fn, sig):
        self.q = q
        self.fn = fn
        self.raw = set()
        self.oth = set()
        self.sig = sig
        self.signaled = False
        self.ev = None
        self.waits = []


class Prog:
    CELLS = {"sb": CELL, "ps": 2048}

    def __init__(self):
        self.all = []
        self.byq = {q: [] for q in ("pe", "act", "dve", "pool", "sp")}
        self.lastw = {"sb": {}, "ps": {}}
        self.readers = {"sb": {}, "ps": {}}
        self.last_sig_pos = {q: -1 for q in self.byq}
        self.cur_tag = "init"

    def _stream(self, ins):
        return ins.q if ins.sig == "eng" or ins.sig is None else ins.sig[1]

    def _cells(self, v):
        cs = self.CELLS[v.space]
        return range(v.lo // cs, (v.hi - 1) // cs + 1)

    def add(self, q, fn, reads=(), writes=(), sig="eng", deps=(), force_signal=None):
        ins = Ins(q, fn, sig)
        ins.epos = len(self.byq[q])
        ins.gidx = len(self.all)
        ins.tag = self.cur_tag
        for v in reads:
            lw, rd = self.lastw[v.space], self.readers[v.space]
            ps_as_write = v.space == "ps"
            for c in self._cells(v):
                w = lw.get(c)
                if w is not None:
                    ins.raw.add(w)
                if ps_as_write:
                    for r in rd.get(c, {}).values():
                        ins.oth.add(r)
                    lw[c] = ins
                    rd[c] = {}
                else:
                    rd.setdefault(c, {})[self._stream(ins)] = ins
        for v in writes:
            lw, rd = self.lastw[v.space], self.readers[v.space]
            for c in self._cells(v):
                w = lw.get(c)
                if w is not None:
                    ins.oth.add(w)
                for r in rd.get(c, {}).values():
                    ins.oth.add(r)
                lw[c] = ins
                rd[c] = {}
        for d in deps:
            ins.raw.add(d)
        ins.raw.discard(ins)
        ins.oth.discard(ins)
        keep = set()
        for d in ins.raw:
            if d.q == ins.q and d.sig == "eng" and ins.sig in ("eng", None) and ins.q == "pe":
                continue
            keep.add(d)
        for d in ins.oth:
            if d.q == ins.q and d.sig == "eng" and ins.sig in ("eng", None):
                continue
            keep.add(d)
        ins.raw = keep
        ins.oth = set()
        if sig == "eng":
            if q != "pe" or force_signal:
                ins.signaled = True
        elif sig is not None:
            ins.signaled = True
        self.all.append(ins)
        self.byq[q].append(ins)
        if ins.signaled and sig == "eng":
            self.last_sig_pos[q] = ins.epos
        for d in ins.raw:
            if d.sig == "eng" and not d.signaled and self.last_sig_pos[d.q] < d.epos:
                last = self.byq[d.q][-1]
                if last.sig != "eng":
                    raise RuntimeError("cannot signal")
                last.signaled = True
                self.last_sig_pos[d.q] = last.epos
        return ins

    def finalize(self):
        self.nextsig = {}
        for q, lst in self.byq.items():
            cnt = 0
            for i in lst:
                if i.sig == "eng" and i.signaled:
                    cnt += 1
                    i.ev = ("E_" + q, cnt)
            nxt = None
            arr = [None] * len(lst)
            for pos in range(len(lst) - 1, -1, -1):
                if lst[pos].sig == "eng" and lst[pos].signaled:
                    nxt = lst[pos]
                arr[pos] = nxt
            self.nextsig[q] = arr
        dmacnt = {}
        for i in self.all:
            if i.sig not in ("eng", None):
                _, sem, amt = i.sig
                dmacnt[sem] = dmacnt.get(sem, 0) + amt
                i.ev = (sem, dmacnt[sem])
        clock = {q: {} for q in self.byq}
        snap = {}
        nwaits = 0
        for i in self.all:
            ck = clock[i.q]
            need = {}
            for d in i.raw:
                if d.sig == "eng":
                    r = self.nextsig[d.q][d.epos]
                    assert r is not None, "unsignaled dependency"
                else:
                    r = d
                s, v = r.ev
                if ck.get(s, 0) >= v:
                    continue
                if need.get(s, (0, None))[0] < v:
                    need[s] = (v, r)
            for s, (v, r) in need.items():
                if ck.get(s, 0) >= v:
                    continue
                i.waits.append((s, v))
                nwaits += 1
                for s2, v2 in snap[r.ev].items():
                    if ck.get(s2, 0) < v2:
                        ck[s2] = v2
            if i.ev is not None:
                sn = dict(ck)
                sn[i.ev[0]] = i.ev[1]
                snap[i.ev] = sn
        self.nwaits = nwaits

    def emit(self, q, e, sems):
        for i in self.byq[q]:
            for s, v in i.waits:
                e.wait_ge(sems[s], v)
            if i.fn is None:
                continue
            bi = i.fn(e)
            if i.ev is not None:
                amt = 1 if i.sig == "eng" else i.sig[2]
                bi.then_inc(sems[i.ev[0]], amt)

    def sem_names(self):
        names = set()
        for i in self.all:
            if i.ev is not None:
                names.add(i.ev[0])
        return sorted(names)


class K:
    def __init__(self, nc, TOK, layers):
        self.nc = nc
        self.TOK = TOK
        self.layers = layers
        self.NBLK = TOK // BLK
        self.P = Prog()
        self.steps = []
        self.bank_rr = 0

    def setup_memory(self, S, PS):
        self.S = S
        self.PS = PS
        off = 0

        def take(nbytes):
            nonlocal off
            o = off
            off += (nbytes + CELL - 1) // CELL * CELL
            return o

        self.o_xt = take(KC * self.TOK * 4)
        self.o_cp = take(CP_COLS * 4)
        self.o_idb = take(512)
        self.o_ghn = take(D * 4)
        self.o_wg = take(512)
        self.o_wb = [take(TILE_EL * 2) for _ in range(NB)]
        self.o_persist = take(1024)
        self.o_arena = off
        self.total = off + self.ARENA
        TOK = self.TOK
        self.xT = self.mk(self.o_xt, F32, [KC, TOK])
        self.cp = self.mk(self.o_cp, F32, [CP_COLS])
        self.identb = self.mk(self.o_idb, BF16, [128])
        self.onesb = self.mk(self.o_idb + 256, BF16, [128])
        self.ghn = self.mk(self.o_ghn, F32, [D])
        self.wgs = [self.mk(self.o_wg, BF16, [KC, 8]), self.mk(self.o_wg + 256, BF16, [KC, 8])]
        self.wb_proj = [self.mk(o, BF16, [KC, 256]) for o in self.o_wb]
        self.wb_down = [self.mk(o, BF16, [16, 128]) for o in self.o_wb]
        pp = self.o_persist
        self.mpv = self.mk(pp, F32, [40])
        self.uprev = self.mk(pp + 256, F32, [KC, 2])
        self.min_ = self.mk(pp + 512, F32, [4])
        self.ident = self.cp[CP_IDENT:CP_IDENT + 128]
        self.maskneg = self.cp[CP_MASK:CP_MASK + 128]
        self.ones = self.cp[CP_ONES:CP_ONES + 128]
        self.neghalf = self.cp[CP_NEGHALF:CP_NEGHALF + 1]
        self.diag4 = self.cp[CP_DIAG4:CP_DIAG4 + 4].p(0, 4)
        self.negdiag4 = self.cp[CP_NEGDIAG4:CP_NEGDIAG4 + 4].p(0, 4)
        self.flag = self.cp[CP_FLAG:CP_FLAG + 1]
        a = self.o_arena
        self.RSTD = self.mk(a, F32, [BLK])
        self.TB = self.mk(a + 2048, F32, [BLK])
        self.SQ = [self.mk(a + 4096, F32, [BLK]), self.mk(a + 6144, F32, [BLK])]
        self.SQB = [self.mk(a + 4096, BF16, [BLK]), self.mk(a + 6144, BF16, [BLK])]
        self.HT = self.mk(a + 8192, BF16, [KC, BLK])
        b = a + 16384
        self.ACTT = self.mk(b, BF16, [16, BLK])
        self.YT_mlp = self.mk(b + 16384, F32, [KC, BLK])
        self.RT = [self.mk(a + 4096, F32, [BLK]), self.mk(a + 6144, F32, [BLK])]
        self.UT = self.mk(b, F32, [KC, BLK + 2])
        self.YT_conv = self.mk(b, F32, [KC, BLK])
        c0 = b + 16640
        self.CSB = [self.mk(c0, F32, [BLK]), self.mk(c0 + 2048, F32, [BLK])]
        self.ACC = [self.mk(c0 + 4096, F32, [BLK]), self.mk(c0 + 6144, F32, [BLK])]
        self.GT = self.mk(c0 + 8192, BF16, [KC, BLK])
        self.ULAST = self.mk(c0 + 16384, F32, [KC, 2])
        self.CSM = self.mk(c0 + 16384 + 256, F32, [16])
        self.HT2 = self.mk(c0 + 16384 + 512, BF16, [KC, 2])
        self.QT = self.mk(b, BF16, [NH, BLK])
        self.KT = self.mk(b + 4096, BF16, [NH, BLK])
        self.VAUG = self.mk(b + 8192, BF16, [4, NH, VA])
        self.VAUG2 = self.mk(b + 8192, BF16, [16, VA])
        self.YT_m = self.mk(b, F32, [KC, BLK])
        m0 = b + 16640
        self.GO = self.mk(m0, BF16, [4, D])
        m1 = m0 + 8192
        self.PT = self.mk(m1, BF16, [NH, CH])
        self.QS = self.mk(m1 + 1024, BF16, [NH, CH])
        self.KW = self.mk(m1 + 2048, BF16, [NH, CH])
        self.HG = self.mk(m1 + 3072, BF16, [D])
        self.OTMP = self.mk(m1 + 3072, F32, [256])
        self.JUNK = self.mk(m1 + 5120, BF16, [256])
        self.TOKS = self.mk(m1 + 5632, F32, [64])
        self.SM = self.mk(m1 + 5888, F32, [64])
        self.R3 = self.mk(m1 + 6144, F32, [64])
        p0 = b + 32768
        self.CST = self.mk(p0, F32, [NH, VA])
        self.CB = self.mk(p0 + 4352, BF16, [NH, VA])
        self.CIN = self.mk(m1, F32, [NH, VA])
        end = p0 + 4352 + 2304
        assert m1 + 6400 <= p0
        assert end - self.o_arena <= self.ARENA, (end - self.o_arena, self.ARENA)
        self.GA = self.RSTD
        self.GB = self.TB
        self.GC = self.SQ[0]
        self.GD = self.SQ[1]
        self.NBD = self.SQ[0]
        self.EB = self.SQ[1]
        self.IB = self.RSTD
        self.banks = [View(PS[:, i * 512:(i + 1) * 512], "ps", i * 2048, [512], [1], 4) for i in range(8)]
        self.banks_b = [View(PS[:, i * 512:(i + 1) * 512].bitcast(BF16), "ps", i * 2048, [1024], [1], 2)
                        for i in range(8)]

    ARENA = 16384 + 32768 + 4352 + 2304

    def mk(self, off, dt, fshape):
        esz = 4 if dt in (F32, F32R) else 2
        n = int(np.prod(fshape))
        nb = n * esz
        n4 = (nb + 3) // 4
        assert off % 4 == 0
        ap = self.S[:, off // 4: off // 4 + n4]
        if esz == 2:
            ap = ap.bitcast(BF16)[:, :n]
        if len(fshape) == 2:
            ap = ap.rearrange("p (a b) -> p a b", a=fshape[0])
        elif len(fshape) == 3:
            ap = ap.rearrange("p (a b c) -> p a b c", a=fshape[0], b=fshape[1])
        strides = []
        s = 1
        for d_ in reversed(fshape):
            strides.append(s)
            s *= d_
        return View(ap, "sb", off, fshape, list(reversed(strides)), esz)

    def bank(self):
        while True:
            self.bank_rr = (self.bank_rr + 1) % 8
            if self.bank_rr != getattr(self, "stats_bank", None):
                return self.bank_rr

    def stats_push(self, j, src):
        if j == 0:
            self.stats_bank = None
            self.stats_bank = self.bank()
            self.stats_pend = None
        self.stats_flush()
        sq = self.SQB[j % 2]
        self.act(sq, src, AF.Square)
        self.stats_pend = (j, sq)

    def stats_flush(self):
        if self.stats_pend is not None:
            j, sq = self.stats_pend
            self.mm(self.banks[self.stats_bank], self.onesb, sq, start=(j == 0), stop=(j == KC - 1))
            self.stats_pend = None

    def mm(self, out, lhsT, rhs, start=True, stop=True, sig=None):
        return self.P.add("pe", lambda e: e.matmul(out.ap, lhsT.ap, rhs.ap, start=start, stop=stop,
                                                   skip_group_check=True),
                          reads=[lhsT, rhs], writes=[out], force_signal=sig)

    def tr(self, out, in_, ident):
        return self.P.add("pe", lambda e: e.transpose(out.ap, in_.ap, ident.ap),
                          reads=[in_, ident], writes=[out])

    def act(self, out, in_, func, bias=None, scale=None, accum=None):
        reads = [in_]
        kw = {}
        if bias is not None:
            if isinstance(bias, View):
                reads.append(bias)
                kw["bias"] = bias.ap
            else:
                kw["bias"] = float(bias)
        if scale is not None:
            if isinstance(scale, View):
                reads.append(scale)
                kw["scale"] = scale.ap
            else:
                kw["scale"] = float(scale)
        writes = [out]
        if accum is not None:
            writes.append(accum)
            kw["accum_out"] = accum.ap
        return self.P.add("act", lambda e: e.activation(out.ap, in_.ap, func, **kw), reads=reads, writes=writes)

    def _eng(self, name):
        return name

    def tt(self, eng, out, in0, in1, op):
        return self.P.add(eng, lambda e: e.tensor_tensor(out.ap, in0.ap, in1.ap, op), reads=[in0, in1], writes=[out])

    def ts(self, eng, out, in0, s1, s2, op0, op1=None):
        reads = [in0]
        a1 = s1.ap if isinstance(s1, View) else s1
        a2 = s2.ap if isinstance(s2, View) else s2
        if isinstance(s1, View):
            reads.append(s1)
        if isinstance(s2, View):
            reads.append(s2)
        if op1 is None:
            return self.P.add(eng, lambda e: e.tensor_scalar(out.ap, in0.ap, a1, None, op0), reads=reads, writes=[out])
        return self.P.add(eng, lambda e: e.tensor_scalar(out.ap, in0.ap, a1, a2, op0, op1), reads=reads, writes=[out])

    def stt(self, out, in0, scalar, in1, op0, op1):
        reads = [in0, in1]
        a = scalar.ap if isinstance(scalar, View) else scalar
        if isinstance(scalar, View):
            reads.append(scalar)
        return self.P.add("dve", lambda e: e.scalar_tensor_tensor(out.ap, in0.ap, a, in1.ap, op0, op1),
                          reads=reads, writes=[out])

    def cp_(self, eng, out, in_):
        if eng == "act":
            return self.act(out, in_, AF.Copy)
        return self.P.add(eng, lambda e: e.tensor_copy(out.ap, in_.ap), reads=[in_], writes=[out])

    def recip(self, out, in_):
        return self.P.add("dve", lambda e: e.reciprocal(out.ap, in_.ap), reads=[in_], writes=[out])

    def scan(self, out, d0, d1, initial, op0, op1):
        reads = [d0, d1]
        a = initial.ap if isinstance(initial, View) else initial
        if isinstance(initial, View):
            reads.append(initial)
        return self.P.add("dve", lambda e: e.tensor_tensor_scan(out.ap, d0.ap, d1.ap, a, op0, op1),
                          reads=reads, writes=[out])

    def memset(self, eng, out, val):
        return self.P.add(eng, lambda e: e.memset(out.ap, val), writes=[out])

    def dma(self, q, out_ap, in_ap, sem, reads=(), writes=(), deps=(), slow=False):
        if slow:
            fn = lambda e: e.dma_start(out=out_ap, in_=in_ap, allow_slow_non_contiguous=True)
        else:
            fn = lambda e: e.dma_start(out=out_ap, in_=in_ap)
        return self.P.add(q, fn, reads=reads, writes=writes, sig=("dma", sem, 16), deps=deps)

    def step(self, tile, fn, tag=None):
        self.steps.append((tile, fn, getattr(self, "tagpfx", "") + (tag or getattr(fn, "__name__", "?"))))

    def run_steps(self):
        tiles = [(i, t) for i, (t, _, _) in enumerate(self.steps) if t is not None]
        issued = 0
        self.wviews = {}
        ti = 0
        for i, (t, fn, tag) in enumerate(self.steps):
            self.P.cur_tag = tag
            if t is not None:
                while issued < len(tiles) and issued < ti + NB:
                    si, tid = tiles[issued]
                    slot = issued % NB
                    deps = [self.cv_dep[tid]]
                    self.dma("sp", self.S[:, self.o_wb[slot] // 4: self.o_wb[slot] // 4 + TILE_EL // 2].bitcast(BF16),
                             self.wsc[tid], "wb%d" % slot, writes=[self.wb_proj[slot]], deps=deps)
                    self.wviews[si] = slot
                    issued += 1
                slot = self.wviews[i]
                ti += 1
                fn(self.wb_proj[slot], self.wb_down[slot])
            else:
                fn()

    def rms_stats(self, srcs, N, rstd, tb):
        bk = self.banks[self.bank()]
        for kc in range(KC):
            sq = self.SQB[kc % 2][0:N]
            self.act(sq, srcs[kc], AF.Square)
            self.mm(bk[0:N], self.onesb, sq, start=(kc == 0), stop=(kc == KC - 1))
        self.act(rstd[0:N], bk[0:N], AF.Ln, bias=self.eps_col, scale=1.0 / D)
        self.act(rstd[0:N], rstd[0:N], AF.Exp, scale=-0.5)

    def gcol(self, l, j, kc):
        c = CP_G + (l * 4 + j) * 8 + kc
        return self.cp[c:c + 1]

    def pre_norm(self, l, j, t0):
        srcs = [self.xT[kc, t0:t0 + BLK] for kc in range(KC)]
        self.rms_stats(srcs, BLK, self.RSTD, self.TB)
        for kc in range(KC):
            self.stt(self.HT[kc], srcs[kc], self.gcol(l, j, kc), self.RSTD, ALU.mult, ALU.mult)

    def post_norm_residual(self, l, j, t0, YT, fuse_pre=None, unnorm_in=False):
        self.stats_flush()
        bk = self.banks[self.stats_bank]
        self.stats_bank = None
        if unnorm_in:
            self.stt(self.RSTD, self.TB, EPS * D, bk, ALU.mult, ALU.add)
            self.act(self.RSTD, self.RSTD, AF.Ln, scale=1.0 / D)
        else:
            self.act(self.RSTD, bk, AF.Ln, bias=self.eps_col, scale=1.0 / D)
        self.act(self.RSTD, self.RSTD, AF.Exp, scale=-0.5)
        if fuse_pre is not None:
            bk2 = self.banks[self.bank()]
        for kc in range(KC):
            self.stt(YT[kc], YT[kc], self.gcol(l, j, kc), self.RSTD, ALU.mult, ALU.mult)
            xs = self.xT[kc, t0:t0 + BLK]
            self.tt("dve", xs, xs, YT[kc], ALU.add)
            if fuse_pre is not None:
                self.act(self.HT[kc], xs, AF.Copy, scale=self.gcol(l, fuse_pre, kc))
                sq = self.SQB[kc % 2]
                self.act(sq, xs, AF.Square)
                self.mm(bk2, self.onesb, sq, start=(kc == 0), stop=(kc == KC - 1))
        if fuse_pre is not None:
            self.act(self.TB, bk2, AF.Identity, bias=self.eps_col, scale=1.0 / D)
            self.tt("dve", self.TB, self.TB, self.TB, ALU.mult)

    def mlp_block(self, l, blk, prefetch=None, lite=None):
        t0 = blk * BLK
        base = l * TILES_PER_LAYER
        for hh in range(2):
            for w in range(8):
                def up(wp, wd, w=w):
                    for cc in range(2):
                        fc = w * 2 + cc
                        bk = self.banks[self.bank()]
                        for kc in range(KC):
                            self.mm(bk, wp[kc, cc * 128:(cc + 1) * 128], self.HT[kc], start=(kc == 0), stop=(kc == KC - 1))
                        rt = self.RT[fc % 2]
                        self.act(rt, bk, AF.Relu)
                        self.tt("dve", self.ACTT[fc], rt, rt, ALU.mult)
                self.step(base + 16 + hh * 8 + w, up)
            if hh == 1 and prefetch is not None:
                self.step(None, prefetch, tag="mix_prenorm")
            for j in range(8):
                def down(wp, wd, j=j, hh=hh):
                    bk = self.banks[self.bank()]
                    for fc in range(16):
                        self.mm(bk, wd[fc], self.ACTT[fc], start=(fc == 0), stop=(fc == 15))
                    if hh == 0:
                        self.act(self.YT_mlp[j], bk, AF.Copy)
                    else:
                        self.tt("dve", self.YT_mlp[j], bk, self.YT_mlp[j], ALU.add)
                        self.stats_push(j, self.YT_mlp[j])
                self.step(base + 32 + hh * 8 + j, down)
        if lite is not None:
            def lite_step():
                self.stats_flush()
                lite()
            self.step(None, lite_step, tag="mix_prenorm")
        self.step(None, lambda: self.post_norm_residual(l, 3, t0, self.YT_mlp, unnorm_in=True), tag="mlp_postnorm")

    def wc(self, lc, j, kc):
        c = CP_WC + (lc * 3 + j) * 8 + kc
        return self.cp[c:c + 1]

    def conv_layer(self, l):
        self.tagpfx = "C."
        lc = l // 2
        base = l * TILES_PER_LAYER
        TOK = self.TOK
        def halo_norm():
            srcs = [self.xT[kc, TOK - 2:TOK] for kc in range(KC)]
            self.rms_stats(srcs, 2, self.RSTD, self.TB)
            for kc in range(KC):
                self.stt(self.HT2[kc], srcs[kc], self.gcol(l, 0, kc), self.RSTD[0:2], ALU.mult, ALU.mult)
        self.step(None, halo_norm)
        hb = {}
        for w in range(8):
            def halo_proj(wp, wd, w=w):
                if w == 0:
                    hb["bk"] = self.banks[self.bank()]
                bk = hb["bk"]
                for cc in range(2):
                    col = (w * 2 + cc) * 2
                    for kc in range(KC):
                        self.mm(bk[col:col + 2], wp[kc, cc * 128:(cc + 1) * 128], self.HT2[kc],
                                start=(kc == 0), stop=(kc == KC - 1))
            self.step(base + 4 + w, halo_proj)

        def halo_finish():
            bk = hb["bk"]
            self.act(self.CSM, bk[0:16], AF.Copy)
            ul = self.ULAST
            self.tt("dve", ul.w(ul.ap.rearrange("p a b -> p (a b)")), self.CSM, bk[16:32], ALU.mult)
            nc = self.nc
            bnc, gth = self.cbounce[lc], self.cgath[lc]
            d1 = self.dma("sp", bnc[:, :], ul.ap.rearrange("p a b -> p (a b)"), "xs", reads=[ul])
            hb["cc"] = self.P.add("pool", lambda e: e.collective_compute(
                "AllGather", ALU.bypass, replica_groups=[[0, 1], [2, 3], [4, 5], [6, 7]],
                ins=[bnc], outs=[gth]), sig=("dma", "cc", 1), deps=[d1])
            self.convert_next(l)
        self.step(None, halo_finish)

        def halo_recv():
            gth = self.cgath[lc]
            up = self.uprev
            self.dma("sp", up.ap.rearrange("p a b -> p (a b)"), gth[0:128, :], "xr", writes=[up], deps=[hb["cc"]])
            self.ts("dve", up, up, self.flag, None, ALU.mult)

        for blk in range(self.NBLK):
            t0 = blk * BLK
            if blk == 0 or not (PREFETCH or PREFETCH_LITE):
                self.step(None, lambda t0=t0: self.pre_norm(l, 0, t0), tag="mix_prenorm")
            st = {}
            for w in range(4):
                def cproj(wp, wd, w=w):
                    for cc in range(2):
                        bk = self.banks[self.bank()]
                        for kc in range(KC):
                            self.mm(bk, wp[kc, cc * 128:(cc + 1) * 128], self.HT[kc], start=(kc == 0), stop=(kc == KC - 1))
                        self.act(self.CSB[cc], bk, AF.Copy)
                self.step(base + 4 + w, cproj)

                def xproj(wp, wd, w=w):
                    for cc in range(2):
                        j = w * 2 + cc
                        bk = self.banks[self.bank()]
                        for kc in range(KC):
                            self.mm(bk, wp[kc, cc * 128:(cc + 1) * 128], self.HT[kc], start=(kc == 0), stop=(kc == KC - 1))
                        self.tt("dve", self.UT[j, 2:BLK + 2], bk, self.CSB[cc], ALU.mult)
                self.step(base + 8 + w, xproj)
            if blk == 0:
                self.step(None, halo_recv)
            for w in range(4):
                def bproj(wp, wd, w=w):
                    if w == 0:
                        self.cp_("dve", self.UT[:, 0:2], self.uprev)
                    for cc in range(2):
                        j = w * 2 + cc
                        bk = self.banks[self.bank()]
                        for kc in range(KC):
                            self.mm(bk, wp[kc, cc * 128:(cc + 1) * 128], self.HT[kc], start=(kc == 0), stop=(kc == KC - 1))
                        acc = self.ACC[cc]
                        self.ts("dve", acc, self.UT[j, 2:BLK + 2], self.wc(lc, 2, j), None, ALU.mult)
                        self.stt(acc, self.UT[j, 1:BLK + 1], self.wc(lc, 1, j), acc, ALU.mult, ALU.add)
                        self.stt(acc, self.UT[j, 0:BLK], self.wc(lc, 0, j), acc, ALU.mult, ALU.add)
                        self.tt("dve", self.GT[j], bk, acc, ALU.mult)
                    if w == 3:
                        self.cp_("dve", self.uprev, self.UT[:, BLK:BLK + 2])
                self.step(base + w, bproj)
            for w in range(4):
                def oproj(wp, wd, w=w):
                    for cc in range(2):
                        j = w * 2 + cc
                        bk = self.banks[self.bank()]
                        for kc in range(KC):
                            self.mm(bk, wp[kc, cc * 128:(cc + 1) * 128], self.GT[kc], start=(kc == 0), stop=(kc == KC - 1))
                        self.act(self.YT_conv[j], bk, AF.Copy)
                        self.stats_push(j, self.YT_conv[j])
                self.step(base + 12 + w, oproj)
            self.step(None, lambda t0=t0: self.post_norm_residual(l, 1, t0, self.YT_conv, fuse_pre=2), tag="mix_postnorm")
            nxt = (lambda t1=t0 + BLK: self.pre_norm(l, 0, t1)) if (PREFETCH and blk + 1 < self.NBLK) else None
            lite = (lambda t1=t0 + BLK: self.pre_norm(l, 0, t1)) if (PREFETCH_LITE and blk + 1 < self.NBLK) else None
            self.mlp_block(l, blk, prefetch=nxt, lite=lite)

    def bgate(self, lm, which):
        c = CP_BG + lm * 2 + which
        return self.cp[c:c + 1].p(0, 4)

    def gate_rows(self, lm, blk, gi_bank, gf_bank, main):
        self.gate_rows_a(lm, blk, gi_bank, gf_bank, main)
        self.gate_rows_b(lm, blk, main)

    def gate_rows_a(self, lm, blk, gi_bank, gf_bank, main):
        A, B, C_, Dd = (self.GA.p(0, 4), self.GB.p(0, 4), self.GC.p(0, 4), self.GD.p(0, 4))
        self.ts("dve", A, gi_bank.p(0, 4), self.bgate(lm, 0), None, ALU.add)
        self.ts("dve", B, gf_bank.p(0, 4), self.bgate(lm, 1), None, ALU.add)
        self.act(A, A, AF.Tanh, scale=1.0 / CAP)
        self.act(B, B, AF.Tanh, scale=1.0 / CAP)
        self.act(B, B, AF.Exp, scale=-CAP)
        self.act(B, B, AF.Ln, bias=self.one_col.p(0, 4))
        ones_row = self.ones.p(0, 4)
        for c in range(4):
            cs = slice(c * CH, (c + 1) * CH)
            self.scan(C_[cs], ones_row, B[cs], 0.0, ALU.mult, ALU.subtract)
        self.stt(A, A, CAP, C_, ALU.mult, ALU.subtract)
        for c in range(4):
            cs = slice(c * CH, (c + 1) * CH)
            gc = blk * 4 + c
            self.scan(B[cs], A[cs], A[cs], self.mpv[gc:gc + 1].p(0, 4), ALU.max, ALU.max)
            self.tt("dve", self.mpv[gc + 1:gc + 2].p(0, 4), C_[c * CH + CH - 1:c * CH + CH], B[c * CH + CH - 1:c * CH + CH], ALU.add)
        A3 = A.ap.rearrange("p (c t) -> p c t", c=4)
        B3 = B.ap.rearrange("p (c t) -> p c t", c=4)
        D3 = Dd.ap.rearrange("p (c t) -> p c t", c=4)
        self.tt("dve", Dd.w(D3), A.w(A3), B.w(B3[:, :, CH - 1:CH].to_broadcast([4, 4, CH])), ALU.subtract)
        self.act(Dd, Dd, AF.Exp)
        if main:
            self.tt("dve", C_, C_, B, ALU.add)
            self.act(C_, C_, AF.Exp, scale=-1.0)

    def gate_rows_b(self, lm, blk, main):
        A, B, C_, Dd = (self.GA.p(0, 4), self.GB.p(0, 4), self.GC.p(0, 4), self.GD.p(0, 4))
        B3 = B.ap.rearrange("p (c t) -> p c t", c=4)
        mb = self.banks[7]
        qs = [(A, 0), (Dd, 1)] + ([(C_, 2)] if main else [])
        for c in range(4):
            for row, qi in qs:
                col = (c * 3 + qi) * 4
                self.mm(mb[col:col + 4], row[c * CH:(c + 1) * CH], self.diag4, start=True, stop=True)
        r3 = self.R3.p(0, 4)
        g0 = blk * 4
        self.tt("dve", r3[0:4], self.mpv[g0:g0 + 4].p(0, 4), B.w(B3[:, :, CH - 1]), ALU.subtract)
        r16 = r3[4:20]
        self.tt("dve", r16.w(r16.ap.rearrange("p (c h) -> p c h", c=4)),
                r3[0:4].w(r3[0:4].ap.unsqueeze(2).to_broadcast([4, 4, 4])),
                self.diag4.w(self.diag4.ap.unsqueeze(1).to_broadcast([4, 4, 4])), ALU.mult)
        self.mm(mb[48:64], self.ones.p(0, 4), r16, start=True, stop=True)
        self.cp_("dve", self.TOKS[0:48], mb[0:48])
        self.act(self.TOKS[48:64], mb[48:64], AF.Exp)

    def tok(self, c, qi, h):
        col = (c * 3 + qi) * 4 + h
        return self.TOKS[col:col + 1]

    def state_update(self, c, main):
        self.state_kw(c)
        self.state_kv(c, main)

    def state_kw(self, c):
        kb = self.banks_b[6]
        for h in range(NH):
            self.tr(kb[h * CH:(h + 1) * CH], self.KT[h, c * CH:(c + 1) * CH], self.identb)
        for h in range(NH):
            if h % 2 == 0:
                self.act(self.KW[h], kb[h * CH:(h + 1) * CH], AF.Copy, scale=self.tok(c, 1, h))
            else:
                self.ts("dve", self.KW[h], kb[h * CH:(h + 1) * CH], self.tok(c, 1, h), None, ALU.mult)

    def state_kv(self, c, main):
        for h in range(NH):
            nb = self.banks[h]
            self.mm(nb[0:VA], self.KW[h], self.VAUG[c, h], start=True, stop=True)
        for h in range(NH):
            nb = self.banks[h]
            il = self.TOKS[48 + c * 4 + h:48 + c * 4 + h + 1]
            self.stt(self.CST[h], self.CST[h], il, nb[0:VA], ALU.mult, ALU.add)
        if main:
            self.cp_("act", self.CB, self.CST)

    def proj_feat(self, wp, dst_fn, evac):
        for cc in range(2):
            bk = self.banks[self.bank()]
            for kc in range(KC):
                self.mm(bk, wp[kc, cc * 128:(cc + 1) * 128], self.HT[kc], start=(kc == 0), stop=(kc == KC - 1))
            evac(cc, bk)

    def proj_tok(self, wp, evac):
        for c in range(4):
            bi = self.bank()
            bk = self.banks[bi]
            half = bk[0:256]
            for kc in range(KC):
                self.mm(half, self.HT[kc, c * CH:(c + 1) * CH], wp[kc], start=(kc == 0), stop=(kc == KC - 1))
            evac(c, half)

    def gates_proj(self, st, lm):
        self.wg = self.wgs[lm]
        gi = self.banks[self.bank()]
        gf = self.banks[self.bank()]
        for kc in range(KC):
            self.mm(gi.p(0, 4), self.wg[kc, 0:4], self.HT[kc], start=(kc == 0), stop=(kc == KC - 1))
        for kc in range(KC):
            self.mm(gf.p(0, 4), self.wg[kc, 4:8], self.HT[kc], start=(kc == 0), stop=(kc == KC - 1))
        st["gi"], st["gf"] = gi, gf

    def mlstm_layer(self, l):
        lm = l // 2
        base = l * TILES_PER_LAYER
        nc = self.nc
        w_in = self.w_in_m

        def layer_init():
            self.dma("sp", self.ghn.ap, self.ghn_d[lm], "ghn", writes=[self.ghn])
            self.ts("dve", self.ghn, self.ghn, 0.5, None, ALU.mult)
            self.memset("dve", self.CST, 0.0)
            self.memset("dve", self.mpv, 0.0)
        self.step(None, layer_init)

        self.tagpfx = "P."
        for blk in range(self.NBLK):
            t0 = blk * BLK
            st = {}
            if blk == 0 or not PREFETCH:
                self.step(None, lambda t0=t0: self.pre_norm(l, 0, t0), tag="mix_prenorm")
            for w in range(2):
                def kproj(wp, wd, w=w):
                    def ev(cc, bk):
                        self.act(self.KT[w * 2 + cc], bk, AF.Copy, scale=128.0 ** -0.5)
                    self.proj_feat(wp, None, ev)
                self.step(base + 2 + w, kproj)
            for w in range(4):
                def vproj(wp, wd, w=w, st=st, blk=blk):
                    if w == 0:
                        self.memset("dve", self.VAUG2[:, 256:257], 1.0)
                        self.gates_proj(st, lm)
                        self.gate_rows_a(lm, blk, st["gi"], st["gf"], main=False)
                    def ev(c, half):
                        self.act(self.VAUG[c, w, 0:256], half, AF.Copy)
                    self.proj_tok(wp, ev)
                self.step(base + 4 + w, vproj)

            def pre_rows(blk=blk):
                self.gate_rows_b(lm, blk, main=False)
            self.step(None, pre_rows, tag="pre_chunks")
            if PREFETCH and blk + 1 < self.NBLK:
                self.step(None, lambda t1=t0 + BLK: self.pre_norm(l, 0, t1), tag="mix_prenorm")

            def pre_chunks():
                for c in range(4):
                    self.state_update(c, main=False)
            self.step(None, pre_chunks)

        xs = {}

        def exchange_send():
            bnc, gth = self.mbounce[lm], self.mgath[lm]
            cflat = self.CST.ap.rearrange("p h v -> p (h v)")
            d1 = self.dma("sp", bnc[:, 0:NH * VA], cflat, "xs", reads=[self.CST])
            gl = self.NBLK * 4
            d2 = self.dma("sp", bnc[:, NH * VA:NH * VA + 4], self.mpv[gl:gl + 4].ap, "xs",
                          reads=[self.mpv[gl:gl + 4]])
            xs["cc"] = self.P.add("pool", lambda e: e.collective_compute(
                "AllGather", ALU.bypass, replica_groups=[[0, 1], [2, 3], [4, 5], [6, 7]],
                ins=[bnc], outs=[gth]), sig=("dma", "cc", 1), deps=[d1, d2])
            self.convert_next(l)
        self.step(None, exchange_send)
        self.tagpfx = "M."

        def exchange_recv():
            gth = self.mgath[lm]
            cc = xs["cc"]
            self.dma("sp", self.CIN.ap.rearrange("p h v -> p (h v)"), gth[0:128, 0:NH * VA], "xr",
                     writes=[self.CIN], deps=[cc])
            self.dma("sp", self.min_[0:1].p(0, 4).ap, gth[0:4, NH * VA:NH * VA + 1], "xr2",
                     writes=[self.min_[0:1]], deps=[cc], slow=True)
            self.ts("dve", self.CST, self.CIN, self.flag, None, ALU.mult)
            self.cp_("act", self.CB, self.CST)
            self.ts("dve", self.mpv[0:1].p(0, 4), self.min_[0:1].p(0, 4), self.flag.p(0, 4), None, ALU.mult)

        for blk in range(self.NBLK):
            t0 = blk * BLK
            st = {}
            if blk == 0 or not (PREFETCH or PREFETCH_LITE):
                self.step(None, lambda t0=t0: self.pre_norm(l, 0, t0), tag="mix_prenorm")
            for w in range(2):
                def qproj(wp, wd, w=w):
                    def ev(cc, bk):
                        self.act(self.QT[w * 2 + cc], bk, AF.Copy)
                    self.proj_feat(wp, None, ev)
                self.step(base + w, qproj)
            for w in range(2):
                def kproj(wp, wd, w=w):
                    def ev(cc, bk):
                        self.act(self.KT[w * 2 + cc], bk, AF.Copy, scale=128.0 ** -0.5)
                    self.proj_feat(wp, None, ev)
                self.step(base + 2 + w, kproj)
            for w in range(4):
                def vproj(wp, wd, w=w, st=st, blk=blk):
                    if w == 0:
                        self.memset("dve", self.VAUG2[:, 256:257], 1.0)
                    def ev(c, half):
                        self.act(self.VAUG[c, w, 0:256], half, AF.Copy)
                    self.proj_tok(wp, ev)
                self.step(base + 4 + w, vproj)
            for w in range(4):
                def oproj(wp, wd, w=w, st=st, blk=blk):
                    if w == 0 and blk > 0:
                        self.gates_proj(st, lm)
                        self.gate_rows_a(lm, blk, st["gi"], st["gf"], main=True)
                    if w == 2 and blk > 0:
                        self.gate_rows_b(lm, blk, main=True)
                        self.chunk_s1(blk, 0)

                    def ev(c, half):
                        tmp = self.OTMP
                        self.act(tmp, half, AF.Tanh, scale=0.5)
                        self.stt(self.GO[c, w * 256:(w + 1) * 256], tmp, 1.0, self.ghn[w * 256:(w + 1) * 256],
                                 ALU.add, ALU.mult)
                    self.proj_tok(wp, ev)
                self.step(base + 8 + w, oproj)

            if blk == 0:
                self.step(None, exchange_recv)

            def chunks(blk=blk, st=st):
                if blk == 0:
                    self.gates_proj(st, lm)
                    self.gate_rows_a(lm, blk, st["gi"], st["gf"], main=True)
                    self.gate_rows_b(lm, blk, main=True)
                    self.chunk_s1(blk, 0)
                for c in range(4):
                    self.chunk_s2a(blk, c)
                    if c < 3:
                        self.chunk_s1(blk, c + 1)
                    self.state_kw(c)
                    self.chunk_s2b(blk, c)
                    if c < 3:
                        self.state_kv(c, main=True)
                        self.chunk_s2c(blk, c)
                    else:
                        self.chunk_s2c(blk, c)
                        self.state_kv(c, main=True)
            self.step(None, chunks)
            for w in range(4):
                def outproj(wp, wd, w=w):
                    for cc in range(2):
                        j = w * 2 + cc
                        bk = self.banks[self.bank()]
                        for kc in range(KC):
                            self.mm(bk, wp[kc, cc * 128:(cc + 1) * 128], self.HT[kc], start=(kc == 0), stop=(kc == KC - 1))
                        self.act(self.YT_m[j], bk, AF.Copy)
                        self.stats_push(j, self.YT_m[j])
                self.step(base + 12 + w, outproj)
            self.step(None, lambda t0=t0: self.post_norm_residual(l, 1, t0, self.YT_m, fuse_pre=2), tag="mix_postnorm")
            nxt = (lambda t1=t0 + BLK: self.pre_norm(l, 0, t1)) if (PREFETCH and blk + 1 < self.NBLK) else None
            lite = (lambda t1=t0 + BLK: self.pre_norm(l, 0, t1)) if (PREFETCH_LITE and blk + 1 < self.NBLK) else None
            self.mlp_block(l, blk, prefetch=nxt, lite=lite)

    def chunk_s1(self, blk, c):
        gc = blk * 4 + c
        cs = slice(c * CH, (c + 1) * CH)
        Mrow = self.GB.p(0, 4)
        nbd = self.NBD.p(0, 4)
        nbd3 = nbd.ap.rearrange("p (h t) -> p h t", h=4)
        self.tt("dve", nbd.w(nbd3), Mrow[cs].w(Mrow[cs].ap.unsqueeze(1).to_broadcast([4, 4, CH])),
                self.negdiag4.w(self.negdiag4.ap.unsqueeze(2).to_broadcast([4, 4, CH])), ALU.mult)
        BC, BC2, SS = self.banks[4], self.banks[5], self.banks[6]
        ones4 = self.ones.p(0, 4)
        self.mm(BC, ones4, nbd, start=True, stop=True)
        self.stt(nbd.w(nbd3), self.diag4.w(self.diag4.ap.unsqueeze(2).to_broadcast([4, 4, CH])),
                 self.mpv[gc:gc + 1].p(0, 4), nbd.w(nbd3), ALU.mult, ALU.add)
        self.mm(BC2, ones4, nbd, start=True, stop=True)
        for h in range(NH):
            self.mm(SS[h * CH:(h + 1) * CH], self.KT[h, cs], self.QT[h, cs], start=True, stop=True)
        E = self.EB
        for h in range(NH):
            self.stt(E[h * CH:(h + 1) * CH], BC[h * CH:(h + 1) * CH], self.tok(c, 0, h), self.maskneg, ALU.add, ALU.min)
        self.act(E, E, AF.Exp)
        self.tt("dve", self.PT.w(self.PT.ap.rearrange("p h t -> p (h t)")), SS, E, ALU.mult)
        self.act(self.IB, BC2, AF.Exp)
        ib3 = self.IB.ap.rearrange("p (h t) -> p h t", h=4)
        self.tt("dve", self.QS, self.QT[:, cs], self.IB.w(ib3), ALU.mult)

    def chunk_s2a(self, blk, c):
        for h in range(NH):
            self.mm(self.banks[h][0:VA], self.PT[h], self.VAUG[c, h], start=True, stop=False)
        for h in range(NH):
            self.mm(self.banks[h][0:VA], self.QS[h], self.CB[h], start=False, stop=True)

    def chunk_s2b(self, blk, c):
        cs = slice(c * CH, (c + 1) * CH)
        sm = self.SM
        dmax, r, ssq, t4, rs2, sc = (sm[0:4], sm[4:8], sm[8:12], sm[12:16], sm[16:20], sm[20:24])
        den_ap = self.PS[:, 0:4 * 512].rearrange("p (b c) -> p b c", b=4)[:, :, 256]
        den = View(den_ap, "ps", 0, [4 * 512], [1], 4)
        fl = self.TOKS[(c * 3 + 2) * 4:(c * 3 + 2) * 4 + 4]
        self.act(dmax, den, AF.Abs)
        for h in range(NH):
            self.act(self.JUNK, self.banks[h][0:256], AF.Square, accum=ssq[h:h + 1])
        self.tt("dve", dmax, dmax, fl, ALU.max)
        self.recip(r, dmax)
        self.tt("dve", t4, r, r, ALU.mult)
        self.tt("dve", t4, t4, ssq, ALU.mult)
        self.act(t4, t4, AF.Ln, bias=self.eps_col, scale=1.0 / 256)
        self.act(rs2, t4, AF.Exp, scale=-0.5)
        self.tt("dve", sc, r, rs2, ALU.mult)
        for h in range(NH):
            self.stt(self.HG[h * 256:(h + 1) * 256], self.banks[h][0:256], sc[h:h + 1],
                     self.GO[c, h * 256:(h + 1) * 256], ALU.mult, ALU.mult)

    def chunk_s2c(self, blk, c):
        cs = slice(c * CH, (c + 1) * CH)
        tb_ = self.banks_b[7]
        for j in range(KC):
            self.tr(tb_[j * CH:(j + 1) * CH], self.HG[j * CH:(j + 1) * CH], self.identb)
        hgt = self.HT[:, cs]
        self.act(hgt, tb_.w(tb_.ap.rearrange("p (j t) -> p j t", j=KC)), AF.Copy)

    def build(self):
        nc = self.nc
        TOK = self.TOK
        dt = nc.dram_tensor
        self.xT_d = dt("xT", [D, TOK], F32, kind="ExternalInput").ap()
        self.cp_d = dt("cpack", [128, CP_COLS], F32, kind="ExternalInput").ap()
        self.ghn_d = dt("ghn", [2, 128, D], F32, kind="ExternalInput").ap()
        self.w_in_m = dt("w_in_mlstm", [2, D, 3080], F32, kind="ExternalInput").ap()
        self.w_out_m = dt("w_out_mlstm", [2, D, D], F32, kind="ExternalInput").ap()
        self.w_in_c = dt("w_in_conv", [2, D, 3072], F32, kind="ExternalInput").ap()
        self.w_out_c = dt("w_out_conv", [2, D, D], F32, kind="ExternalInput").ap()
        self.w_up = dt("w_mlp_up", [4, D, DFF], F32, kind="ExternalInput").ap()
        self.w_dn = dt("w_mlp_down", [4, DFF, D], F32, kind="ExternalInput").ap()
        self.yT_d = dt("yT", [D, TOK], F32, kind="ExternalOutput").ap()
        self.wsc = dt("wsc", [4 * TILES_PER_LAYER, 128, TILE_EL], BF16, kind="Internal").ap()
        self.mbounce = [dt("mb%d" % i, [128, 1032], F32, kind="Internal").ap() for i in range(2)]
        self.mgath = [dt("mg%d" % i, [256, 1032], F32, kind="Internal").ap() for i in range(2)]
        self.cbounce = [dt("cb%d" % i, [128, 16], F32, kind="Internal").ap() for i in range(2)]
        self.cgath = [dt("cg%d" % i, [256, 16], F32, kind="Internal").ap() for i in range(2)]

        with contextlib.ExitStack() as es:
            tot_probe = KC * TOK * 4
            self.S = None
            dummy_total = self._layout_total()
            S = es.enter_context(nc.sbuf_tensor("S", [128, dummy_total // 4], F32))
            PS = es.enter_context(nc.psum_tensor("PS", [128, 4096], F32))
            self.setup_memory(S, PS)
            assert self.total == dummy_total
            self.eps_col = self.mk(self.o_persist + 768, F32, [1])
            self.one_col = self.mk(self.o_persist + 772, F32, [1])
            self.program()
            self.P.finalize()
            sems = {n: es.enter_context(nc.semaphore(n)) for n in self.P.sem_names()}
            block = es.enter_context(nc.Block())
            P = self.P

            @block.tensor
            def _(e):
                P.emit("pe", e, sems)

            @block.scalar
            def _(e):
                P.emit("act", e, sems)

            @block.vector
            def _(e):
                P.emit("dve", e, sems)

            @block.gpsimd
            def _(e):
                P.emit("pool", e, sems)

            @block.sync
            def _(e):
                P.emit("sp", e, sems)
        return nc

    def _layout_total(self):
        off = 0

        def take(nbytes):
            nonlocal off
            off += (nbytes + CELL - 1) // CELL * CELL
        take(KC * self.TOK * 4)
        take(CP_COLS * 4)
        take(512)
        take(D * 4)
        take(512)
        for _ in range(NB):
            take(TILE_EL * 2)
        take(1024)
        return off + self.ARENA

    def convert_layer(self, l):
        lidx = l // 2
        base = l * TILES_PER_LAYER
        if l % 2 == 0:
            win, wout = self.w_in_m[lidx], self.w_out_m[lidx]
        else:
            win, wout = self.w_in_c[lidx], self.w_out_c[lidx]
        winr = win.rearrange("(kc p) c -> p kc c", p=128)
        woutr = wout.rearrange("(kc p) c -> p kc c", p=128)
        wupr = self.w_up[l].rearrange("(kc p) c -> p kc c", p=128)
        wdnr = self.w_dn[l].rearrange("(fc p) c -> p fc c", p=128)
        first = (l == self.layers[0]) and l % 2 == 0
        order = ([2, 3, 4, 5, 6, 7] + [t for t in range(TILES_PER_LAYER) if t not in (2, 3, 4, 5, 6, 7)]) if first \
            else list(range(TILES_PER_LAYER))
        ga = []
        for n_, t in enumerate(order):
            if t < 12:
                src = winr[:, :, t * 256:(t + 1) * 256]
                dst = self.wsc[base + t].rearrange("p (a b) -> p a b", a=KC)
            elif t < 16:
                src = woutr[:, :, (t - 12) * 256:(t - 11) * 256]
                dst = self.wsc[base + t].rearrange("p (a b) -> p a b", a=KC)
            elif t < 32:
                src = wupr[:, :, (t - 16) * 256:(t - 15) * 256]
                dst = self.wsc[base + t].rearrange("p (a b) -> p a b", a=KC)
            else:
                hh, j = divmod(t - 32, 8)
                src = wdnr[:, hh * 16:(hh + 1) * 16, j * 128:(j + 1) * 128]
                dst = self.wsc[base + t].rearrange("p (a b) -> p a b", a=16)
            early = first and n_ < 6
            d_ = self.dma("pool", dst, src, ("cva%d" if early else "cv%d") % l)
            if early:
                ga.append(base + t)
                for tt_ in ga:
                    self.cv_dep[tt_] = d_
            else:
                for t2 in order[(6 if first else 0):]:
                    self.cv_dep[base + t2] = d_

    def convert_next(self, l):
        i = self.layers.index(l)
        if i + 1 < len(self.layers):
            self.convert_layer(self.layers[i + 1])

    def program(self):
        TOK = self.TOK
        self.dma("sp", self.cp.ap, self.cp_d, "cpl", writes=[self.cp])
        xr = self.xT_d.rearrange("(kc p) t -> p kc t", p=128)
        for kc in range(KC):
            self.dma("sp", self.xT[kc].ap, xr[:, kc, :], "xl%d" % kc, writes=[self.xT[kc]])
        self.cp_("dve", self.identb, self.ident)
        self.cp_("dve", self.onesb, self.ones)
        self.memset("dve", self.eps_col, EPS)
        self.memset("dve", self.one_col, 1.0)
        for lm in range(2):
            if 2 * lm in self.layers:
                self.dma("pool", self.wgs[lm].ap,
                         self.w_in_m[lm].rearrange("(kc p) c -> p kc c", p=128)[:, :, 3072:3080], "wgl%d" % lm,
                         writes=[self.wgs[lm]])
        self.cv_dep = {}
        self.convert_layer(self.layers[0])
        for l in self.layers:
            if l % 2 == 0:
                self.mlstm_layer(l)
            else:
                self.conv_layer(l)
        self.run_steps()
        yr = self.yT_d.rearrange("(kc p) t -> p kc t", p=128)
        outs = []
        for kc in range(KC):
            outs.append(self.dma("sp", yr[:, kc, :], self.xT[kc].ap, "xl%d" % kc, reads=[self.xT[kc]]))
        self.P.add("sp", None, sig=None, deps=outs)


def build_nc(TOK, layers):
    nc = bass.Bass("TRN2", target_bir_lowering=False)
    k = K(nc, TOK, layers)
    k.build()
    return nc, k


def make_cpack(norm_g, w_conv, b_gates, flag):
    cp = np.zeros((128, CP_COLS), np.float32)
    cp[:, CP_IDENT:CP_IDENT + 128] = np.eye(128, dtype=np.float32)
    s = np.arange(128)[:, None]
    t = np.arange(128)[None, :]
    cp[:, CP_MASK:CP_MASK + 128] = np.where(s <= t, 0.0, -30000.0).astype(np.float32)
    cp[:, CP_ONES:CP_ONES + 128] = 1.0
    cp[:, CP_G:CP_G + 128] = norm_g.reshape(4, 4, KC, 128).transpose(3, 0, 1, 2).reshape(128, 128)
    cp[:, CP_WC:CP_WC + 48] = w_conv.reshape(2, 3, KC, 128).transpose(3, 0, 1, 2).reshape(128, 48)
    cp[:, CP_NEGHALF] = -0.5
    cp[0:4, CP_DIAG4:CP_DIAG4 + 4] = np.eye(4, dtype=np.float32)
    cp[0:4, CP_NEGDIAG4:CP_NEGDIAG4 + 4] = -np.eye(4, dtype=np.float32)
    for lm in range(2):
        cp[0:4, CP_BG + lm * 2 + 0] = b_gates[lm, 0:4]
        cp[0:4, CP_BG + lm * 2 + 1] = b_gates[lm, 4:8]
    cp[:, CP_FLAG] = flag
    return cp


_CACHE = {}
LAST = {}


def run(x, norm_g, w_in_mlstm, b_gates_mlstm, g_hnorm, w_out_mlstm, w_in_conv, w_conv, w_out_conv,
        w_mlp_up, w_mlp_down, layers=(0, 1, 2, 3), trace=False):
    B, S, _ = x.shape
    assert B * 2 == NCORES
    TOK = S // 2
    key = (TOK, tuple(layers))
    if key not in _CACHE:
        _CACHE[key] = build_nc(TOK, list(layers))[0]
    nc = _CACHE[key]
    f = lambda a: np.ascontiguousarray(np.asarray(a, dtype=np.float32))
    ghn = np.ascontiguousarray(np.broadcast_to(f(g_hnorm)[:, None, :], (2, 128, D)))
    shared = {
        "ghn": ghn,
        "w_in_mlstm": f(w_in_mlstm), "w_out_mlstm": f(w_out_mlstm),
        "w_in_conv": f(w_in_conv), "w_out_conv": f(w_out_conv),
        "w_mlp_up": f(w_mlp_up), "w_mlp_down": f(w_mlp_down),
    }
    xf = f(x)
    in_maps = []
    for c in range(NCORES):
        b, half = divmod(c, 2)
        m = dict(shared)
        m["xT"] = np.ascontiguousarray(xf[b, half * TOK:(half + 1) * TOK, :].T)
        m["cpack"] = make_cpack(f(norm_g), f(w_conv), f(b_gates_mlstm), float(half))
        in_maps.append(m)
    res = run_bass_kernel_spmd(nc, in_maps, core_ids=list(range(NCORES)), **({"trace": True} if trace else {}))
    LAST["exec_ns"] = getattr(res, "exec_time_ns", None)
    out = np.empty((B, S, D), np.float32)
    for c in range(NCORES):
        b, half = divmod(c, 2)
        out[b, half * TOK:(half + 1) * TOK, :] = res.results[c]["yT"].T
    return out


def kernel(x, norm_g, w_in_mlstm, b_gates_mlstm, g_hnorm, w_out_mlstm, w_in_conv, w_conv, w_out_conv,
           w_mlp_up, w_mlp_down):
    return run(x, norm_g, w_in_mlstm, b_gates_mlstm, g_hnorm, w_out_mlstm, w_in_conv, w_conv, w_out_conv,
               w_mlp_up, w_mlp_down)
```
